# Optimizing a Trainium2 kernel written in Bass

```python
import jax, jax.numpy as jnp
from jax import lax
import numpy as np

D_MODEL = 2048
BATCH = 1
SEQ = 16384
DEPTH = 2

N_A_LAYERS = DEPTH // 2
N_B_LAYERS = DEPTH - N_A_LAYERS
HEAD_DIM = 128
MIX_WIDTH = D_MODEL
MEM_TOKENS = 256
MEM_HEADS = 4
MEM_WIDTH = MEM_HEADS * HEAD_DIM
LRU_WIDTH = MIX_WIDTH - MEM_WIDTH
LRU_BLOCKS = LRU_WIDTH // HEAD_DIM
LRU_BLOCK = LRU_WIDTH // LRU_BLOCKS
CONV_WIDTH = 4
LRU_C = 8.0
SB_HEADS = (MIX_WIDTH - MEM_WIDTH) // HEAD_DIM
SB_WIDTH = SB_HEADS * HEAD_DIM
Q_BLOCK = 128
N_EXPERTS = 32
TOP_K = 4
D_FF = D_MODEL
SWIGLU_LIMIT = 7.0
SWIGLU_ALPHA = 1.702
EXPERT_CHUNK = 512
DN_ALPHA = (2 * DEPTH) ** 0.25
DN_BETA = (8 * DEPTH) ** -0.25
LN_EPS = 1e-5

kernel_name = "hybrid_rglru_stickbreak_moe_deepnorm"

F32 = jnp.float32


def layer_norm(h, g, b):
    hf = h.astype(F32)
    mu = jnp.mean(hf, axis=-1, keepdims=True)
    var = jnp.mean(jnp.square(hf - mu), axis=-1, keepdims=True)
    y = (hf - mu) * lax.rsqrt(var + LN_EPS) * g.astype(F32) + b.astype(F32)
    return y.astype(h.dtype)


def memory_attention(q, mem_kv):
    B, S, _ = q.shape
    M = mem_kv.shape[1]
    qh = q.reshape(B, S, MEM_HEADS, HEAD_DIM)
    k = mem_kv[..., :MEM_WIDTH].reshape(B, M, MEM_HEADS, HEAD_DIM)
    v = mem_kv[..., MEM_WIDTH:].reshape(B, M, MEM_HEADS, HEAD_DIM)
    s = jnp.einsum('bshd,bmhd->bhsm', qh.astype(F32), k.astype(F32)) * (HEAD_DIM ** -0.5)
    p = jax.nn.softmax(s, axis=-1)
    o = jnp.einsum('bhsm,bmhd->bshd', p, v.astype(F32))
    return o.reshape(B, S, MEM_WIDTH).astype(q.dtype)


def causal_depthwise_conv(u, w, bias):
    S = u.shape[1]
    up = jnp.pad(u, ((0, 0), (CONV_WIDTH - 1, 0), (0, 0)))
    out = bias
    for tap in range(CONV_WIDTH):
        out = out + up[:, tap:tap + S] * w[tap]
    return out


def _linear_recurrence_combine(left, right):
    a_l, b_l = left
    a_r, b_r = right
    return a_l * a_r, a_r * b_l + b_r


def rg_lru(xc, rg_w, rg_b, ig_w, ig_b, lam):
    B, S, W = xc.shape
    xf = xc.astype(F32)
    xb = xf.reshape(B, S, LRU_BLOCKS, LRU_BLOCK)
    r = jax.nn.sigmoid(jnp.einsum('bsnc,ncd->bsnd', xb, rg_w.astype(F32)).reshape(B, S, W) + rg_b.astype(F32))
    i = jax.nn.sigmoid(jnp.einsum('bsnc,ncd->bsnd', xb, ig_w.astype(F32)).reshape(B, S, W) + ig_b.astype(F32))
    log_a = -LRU_C * r * jax.nn.softplus(-lam.astype(F32))
    a = jnp.exp(log_a)
    b = jnp.sqrt(-jnp.expm1(2.0 * log_a)) * (i * xf)
    _, h = lax.associative_scan(_linear_recurrence_combine, (a, b), axis=1)
    return h.astype(xc.dtype)


def rglru_layer_mixer(h, mem_kv, w_in, conv_w, conv_b, rg_w, rg_b, ig_w, ig_b, lam, w_out):
    proj = h @ w_in
    gate_branch = proj[..., :LRU_WIDTH]
    lru_in = proj[..., LRU_WIDTH:2 * LRU_WIDTH]
    q_mem = proj[..., 2 * LRU_WIDTH:]
    xc = causal_depthwise_conv(lru_in, conv_w, conv_b)
    lru_out = rg_lru(xc, rg_w, rg_b, ig_w, ig_b, lam) * jax.nn.gelu(gate_branch)
    mem_out = memory_attention(q_mem, mem_kv)
    return jnp.concatenate([lru_out, mem_out], axis=-1) @ w_out


def stick_breaking_attention(q, k, v):
    B, S, H, d = q.shape
    n_blk = S // Q_BLOCK
    qb = q.astype(F32).reshape(B, n_blk, Q_BLOCK, H, d).transpose(1, 0, 3, 2, 4)
    kf = k.astype(F32).transpose(0, 2, 1, 3)
    vf = v.astype(F32).transpose(0, 2, 1, 3)
    kpos = jnp.arange(S)
    scale = d ** -0.5

    def one_block(args):
        qi, q0 = args
        z = jnp.einsum('bhqd,bhkd->bhqk', qi, kf) * scale
        qpos = q0 + jnp.arange(Q_BLOCK)
        causal = kpos[None, :] < qpos[:, None]
        log_keep = jnp.where(causal, jax.nn.log_sigmoid(-z), 0.0)
        after = lax.cumsum(log_keep, axis=3, reverse=True) - log_keep
        wts = jnp.where(causal, jnp.exp(jax.nn.log_sigmoid(z) + after), 0.0)
        return jnp.einsum('bhqk,bhkd->bqhd', wts, vf)

    out = lax.map(one_block, (qb, jnp.arange(n_blk) * Q_BLOCK))
    return out.transpose(1, 0, 2, 3, 4).reshape(B, S, H * d).astype(q.dtype)


def stickbreak_layer_mixer(h, kv_shared, mem_kv, w_q, w_out):
    B, S, _ = h.shape
    proj = h @ w_q
    q_sb = proj[..., :SB_WIDTH].reshape(B, S, SB_HEADS, HEAD_DIM)
    q_mem = proj[..., SB_WIDTH:]
    k = kv_shared[..., :SB_WIDTH].reshape(B, S, SB_HEADS, HEAD_DIM)
    v = kv_shared[..., SB_WIDTH:].reshape(B, S, SB_HEADS, HEAD_DIM)
    sb_out = stick_breaking_attention(q_sb, k, v)
    mem_out = memory_attention(q_mem, mem_kv)
    return jnp.concatenate([sb_out, mem_out], axis=-1) @ w_out


def moe_ffn(h, router_w, router_b, w_gate_up, b_gate_up, w_down, b_down):
    B, S, D = h.shape
    tok = h.reshape(-1, D)
    T = tok.shape[0]
    TK = T * TOP_K
    logits = (tok @ router_w).astype(F32) + router_b.astype(F32)
    top_v, top_e = lax.top_k(logits, TOP_K)
    gates = jax.nn.softmax(top_v, axis=-1)
    flat_e = top_e.reshape(-1).astype(jnp.int32)
    flat_tok = (jnp.arange(TK, dtype=jnp.int32) // TOP_K)
    flat_g = gates.reshape(-1)
    order = jnp.argsort(flat_e)
    s_e, s_tok, s_g = flat_e[order], flat_tok[order], flat_g[order]
    counts = jnp.bincount(flat_e, length=N_EXPERTS).astype(jnp.int32)
    padded = (counts + EXPERT_CHUNK - 1) // EXPERT_CHUNK * EXPERT_CHUNK
    group_start = jnp.cumsum(counts) - counts
    padded_end = jnp.cumsum(padded)
    padded_start = padded_end - padded
    dest = padded_start[s_e] + jnp.arange(TK, dtype=jnp.int32) - group_start[s_e]
    n_chunks = -(-TK // EXPERT_CHUNK) + N_EXPERTS
    buf_tok = jnp.zeros((n_chunks * EXPERT_CHUNK,), jnp.int32).at[dest].set(s_tok)
    buf_g = jnp.zeros((n_chunks * EXPERT_CHUNK,), F32).at[dest].set(s_g)
    chunk_e = jnp.minimum(
        jnp.searchsorted(padded_end, jnp.arange(n_chunks, dtype=jnp.int32) * EXPERT_CHUNK, side='right'),
        N_EXPERTS - 1)

    def run_chunk(args):
        idx, g, e = args
        xe = tok[idx]
        gu = (xe @ w_gate_up[e] + b_gate_up[e]).astype(F32)
        gate = jnp.minimum(gu[..., 0::2], SWIGLU_LIMIT)
        up = jnp.clip(gu[..., 1::2], -SWIGLU_LIMIT, SWIGLU_LIMIT)
        act = (up + 1.0) * gate * jax.nn.sigmoid(SWIGLU_ALPHA * gate)
        y = (act.astype(xe.dtype) @ w_down[e] + b_down[e]).astype(F32)
        return y * g[:, None]

    rows = lax.map(run_chunk, (buf_tok.reshape(n_chunks, EXPERT_CHUNK),
                               buf_g.reshape(n_chunks, EXPERT_CHUNK), chunk_e))
    out = jnp.zeros((T, D), F32).at[buf_tok].add(rows.reshape(-1, D))
    return out.reshape(B, S, D).astype(h.dtype)


def setup_inputs(seed: int = 0) -> dict:
    key = jax.random.key(seed)
    ks = jax.random.split(key, 32)
    D = D_MODEL

    def nrm(k, shape, scale):
        return jax.random.normal(k, shape, F32) * scale

    a_u = jax.random.uniform(ks[10], (N_A_LAYERS, LRU_WIDTH), F32, 0.9, 0.999)
    a_base = a_u ** (1.0 / LRU_C)
    return {
        "x": nrm(ks[0], (BATCH, SEQ, D), 1.0),
        "mem": nrm(ks[1], (BATCH, MEM_TOKENS, D), 1.0),
        "a_w_in": nrm(ks[2], (N_A_LAYERS, D, 2 * LRU_WIDTH + MEM_WIDTH), D ** -0.5),
        "a_conv_w": nrm(ks[3], (N_A_LAYERS, CONV_WIDTH, LRU_WIDTH), CONV_WIDTH ** -0.5),
        "a_conv_b": nrm(ks[4], (N_A_LAYERS, LRU_WIDTH), 0.01),
        "a_rg_w": nrm(ks[5], (N_A_LAYERS, LRU_BLOCKS, LRU_BLOCK, LRU_BLOCK), LRU_BLOCK ** -0.5),
        "a_rg_b": nrm(ks[6], (N_A_LAYERS, LRU_WIDTH), 0.01),
        "a_ig_w": nrm(ks[7], (N_A_LAYERS, LRU_BLOCKS, LRU_BLOCK, LRU_BLOCK), LRU_BLOCK ** -0.5),
        "a_ig_b": nrm(ks[8], (N_A_LAYERS, LRU_WIDTH), 0.01),
        "a_lambda": jnp.log(a_base) - jnp.log1p(-a_base),
        "a_w_out": nrm(ks[9], (N_A_LAYERS, LRU_WIDTH + MEM_WIDTH, D), (LRU_WIDTH + MEM_WIDTH) ** -0.5 * DN_BETA),
        "b_w_q": nrm(ks[11], (N_B_LAYERS, D, SB_WIDTH + MEM_WIDTH), D ** -0.5),
        "b_w_out": nrm(ks[12], (N_B_LAYERS, SB_WIDTH + MEM_WIDTH, D), (SB_WIDTH + MEM_WIDTH) ** -0.5 * DN_BETA),
        "w_kv_shared": nrm(ks[13], (D, 2 * SB_WIDTH), D ** -0.5),
        "mem_w_kv": nrm(ks[14], (DEPTH, D, 2 * MEM_WIDTH), D ** -0.5),
        "ln1_g": 1.0 + nrm(ks[15], (DEPTH, D), 0.02),
        "ln1_b": nrm(ks[16], (DEPTH, D), 0.02),
        "ln2_g": 1.0 + nrm(ks[17], (DEPTH, D), 0.02),
        "ln2_b": nrm(ks[18], (DEPTH, D), 0.02),
        "router_w": nrm(ks[19], (DEPTH, D, N_EXPERTS), D ** -0.5),
        "router_b": nrm(ks[20], (DEPTH, N_EXPERTS), 0.01),
        "w_gate_up": nrm(ks[21], (DEPTH, N_EXPERTS, D, 2 * D_FF), D ** -0.5),
        "b_gate_up": nrm(ks[22], (DEPTH, N_EXPERTS, 2 * D_FF), 0.01),
        "w_down": nrm(ks[23], (DEPTH, N_EXPERTS, D_FF, D), D_FF ** -0.5 * DN_BETA),
        "b_down": nrm(ks[24], (DEPTH, N_EXPERTS, D), 0.01),
    }


def reference(x, mem, a_w_in, a_conv_w, a_conv_b, a_rg_w, a_rg_b, a_ig_w, a_ig_b, a_lambda, a_w_out,
              b_w_q, b_w_out, w_kv_shared, mem_w_kv, ln1_g, ln1_b, ln2_g, ln2_b,
              router_w, router_b, w_gate_up, b_gate_up, w_down, b_down):
    h = x
    kv_shared = None
    for layer in range(DEPTH):
        mem_kv = mem @ mem_w_kv[layer]
        if layer < N_A_LAYERS:
            i = layer
            mix = rglru_layer_mixer(h, mem_kv, a_w_in[i], a_conv_w[i], a_conv_b[i], a_rg_w[i], a_rg_b[i],
                                    a_ig_w[i], a_ig_b[i], a_lambda[i], a_w_out[i])
        else:
            if layer == N_A_LAYERS:
                kv_shared = h @ w_kv_shared
            j = layer - N_A_LAYERS
            mix = stickbreak_layer_mixer(h, kv_shared, mem_kv, b_w_q[j], b_w_out[j])
        h = layer_norm(DN_ALPHA * h + mix, ln1_g[layer], ln1_b[layer])
        ffn = moe_ffn(h, router_w[layer], router_b[layer], w_gate_up[layer], b_gate_up[layer],
                      w_down[layer], b_down[layer])
        h = layer_norm(DN_ALPHA * h + ffn, ln2_g[layer], ln2_b[layer])
    return h
```

```python
import numpy as np
from contextlib import ExitStack
import concourse.bass as bass
import concourse.mybir as mybir
from concourse.bass_utils import run_bass_kernel_spmd

F32 = mybir.dt.float32
BF16 = mybir.dt.bfloat16
AF = mybir.ActivationFunctionType
ALU = mybir.AluOpType
AX = mybir.AxisListType

NCORES = 8
D = 2048
S = 16384
T = S // NCORES
NCH = D // 128
NE = 32
TOPK = 4
DN_ALPHA = 4.0 ** 0.25
LN_EPS = 1e-5
LRU_W = 1536
MEM_W = 512
NMEM = 256
EPOCH = 30000


class Buf:
    __slots__ = ("name", "w", "r")

    def __init__(self, name=""):
        self.name = name
        self.w = None
        self.r = []


class KB:
    def __init__(self, nc, stack, n_dma_sems=48):
        self.nc = nc
        self.stack = stack
        self.eng = {"pe": nc.tensor, "dve": nc.vector, "act": nc.scalar, "pool": nc.gpsimd, "sp": nc.sync}
        self.cnt = {e: 0 for e in self.eng}
        self.sems = {e: [] for e in self.eng}
        self.seen = {e: {} for e in self.eng}
        self.cc_sem = stack.enter_context(nc.semaphore("cc_sem"))
        self.cc_val = 0
        self.cc_scratch = stack.enter_context(nc.sbuf_tensor("cc_scratch", [128, 8], F32))
        self.dma_sems = [stack.enter_context(nc.semaphore(f"dq{i}")) for i in range(n_dma_sems)]
        self.dma_val = [0] * n_dma_sems
        self.dma_rr = 0
        self.n_inst = 0

    def _eng_sem(self, e, count):
        ep = (count - 1) // EPOCH
        while len(self.sems[e]) <= ep:
            self.sems[e].append(self.stack.enter_context(self.nc.semaphore(f"s_{e}_{len(self.sems[e])}")))
        return self.sems[e][ep], count - ep * EPOCH

    def _wait(self, e, tag):
        kind, key, value = tag
        if kind == "eng":
            if key == e and e in ("pe", "sp"):
                return
            sem, v = self._eng_sem(key, value)
            sid = ("eng", key, (value - 1) // EPOCH)
        elif kind == "cc":
            sem, v = self.cc_sem, value
            sid = ("cc", 0)
        else:
            sem, v = self.dma_sems[key], value
            sid = ("dma", key)
        if self.seen[e].get(sid, 0) >= v:
            return
        self.seen[e][sid] = v
        self.eng[e].wait_ge(sem, v)
        self.n_inst += 1

    def _deps(self, e, reads, writes):
        for b in reads:
            if b.w is not None:
                self._wait(e, b.w)
        for b in writes:
            if b.w is not None:
                self._wait(e, b.w)
            for t in b.r:
                self._wait(e, t)

    def _commit(self, tag, reads, writes):
        for b in reads:
            b.r.append(tag)
            if len(b.r) > 48:
                last = {}
                for t in b.r:
                    last[(t[0], t[1])] = t
                b.r = list(last.values())
        for b in writes:
            b.w = tag
            b.r = []

    def op(self, e, fn, reads=(), writes=()):
        self._deps(e, reads, writes)
        self.cnt[e] += 1
        sem, v = self._eng_sem(e, self.cnt[e])
        ins = fn(self.eng[e])
        ins.then_inc(sem, 1)
        self.n_inst += 1
        self._commit(("eng", e, self.cnt[e]), reads, writes)
        return ins

    def dma(self, q, out, in_, reads=(), writes=(), **kw):
        self._deps(q, reads, writes)
        k = self.dma_rr
        self.dma_rr = (self.dma_rr + 1) % len(self.dma_sems)
        if self.dma_val[k] > 0:
            self._wait(q, ("dma", k, self.dma_val[k]))
        self.dma_val[k] += 16
        ins = self.eng[q].dma_start(out=out, in_=in_, **kw)
        ins.then_inc(self.dma_sems[k], 16)
        self.n_inst += 1
        tag = ("dma", k, self.dma_val[k])
        self._commit(tag, reads, writes)
        return tag

    def collective(self, kind, ins, outs, reads=(), writes=(), groups=None):
        q = "pool"
        self._deps(q, reads, writes)
        self.cc_val += 1
        ins_ = self.eng[q].collective_compute(kind, ALU.bypass, replica_groups=groups or [list(range(NCORES))],
                                              ins=[a.opt() for a in ins], outs=[a.opt() for a in outs])
        ins_.then_inc(self.cc_sem)
        self.eng[q].wait_ge(self.cc_sem, self.cc_val)
        self.n_inst += 2
        self.cnt[q] += 1
        sem, v = self._eng_sem(q, self.cnt[q])
        self.eng[q].memset(self.cc_scratch[:], 0.0).then_inc(sem, 1)
        tag = ("eng", q, self.cnt[q])
        self._commit(tag, reads, writes)
        return tag

    def barrier(self):
        for e in self.eng:
            for o in self.eng:
                if o != e and self.cnt[o] > 0:
                    self._wait(e, ("eng", o, self.cnt[o]))
            for k, v in enumerate(self.dma_val):
                if v > 0:
                    self._wait(e, ("dma", k, v))


class Ctx:
    def __init__(self, nc, kb, stack):
        self.nc, self.kb, self.stack = nc, kb, stack
        self.psum = []
        for i in range(8):
            t = stack.enter_context(nc.psum_tensor(f"ps{i}", [128, 512], F32))
            self.psum.append((t, Buf(f"ps{i}")))

    def sb(self, st, name, shape, dt=F32):
        self.uid = getattr(self, "uid", 0) + 1
        return st.enter_context(self.nc.sbuf_tensor(f"sb{self.uid}_{name}", shape, dt))


def load_consts(cx, st, dr):
    kb = cx.kb
    c = {}
    c["ident"] = cx.sb(st, "ident", [128, 128]); c["b_ident"] = Buf()
    kb.dma("sp", c["ident"][:], dr["ident"], writes=[c["b_ident"]])
    c["ones"] = cx.sb(st, "ones", [128, 128]); c["b_ones"] = Buf()
    kb.op("pool", lambda e: e.memset(c["ones"][:], 1.0), writes=[c["b_ones"]])
    c["ones_bf"] = cx.sb(st, "ones_bf", [128, 128], BF16); c["b_ones_bf"] = Buf()
    kb.op("pool", lambda e: e.memset(c["ones_bf"][:], 1.0), writes=[c["b_ones_bf"]])
    return c


def emit_ln(cx, c, ws, v_aps, v_bufs, g_ap, b_ap, gb_buf, out_dram, out_buf, tsl, psA, psB):
    kb = cx.kb
    sq, b_sq = ws["sq"], ws["b_sq"]
    (pA, bA), (pB, bB) = psA, psB
    for ch in range(NCH):
        k = ch % 2
        kb.op("act", lambda e, ch=ch, k=k: e.activation(sq[k][:], v_aps[ch], AF.Square), reads=[v_bufs[ch]], writes=[b_sq[k]])
        kb.op("pe", lambda e, ch=ch: e.matmul(pA[:], lhsT=c["ones"][:], rhs=v_aps[ch], start=(ch == 0), stop=(ch == NCH - 1)),
              reads=[c["b_ones"], v_bufs[ch]], writes=[bA])
        kb.op("pe", lambda e, ch=ch, k=k: e.matmul(pB[:], lhsT=c["ones"][:], rhs=sq[k][:], start=(ch == 0), stop=(ch == NCH - 1)),
              reads=[c["b_ones"], b_sq[k]], writes=[bB])
    mean, b_mean = ws["mean"], ws["b_mean"]
    rstd, b_rstd = ws["rstd"], ws["b_rstd"]
    kb.op("act", lambda e: e.mul(mean[:], pA[:], 1.0 / D), reads=[bA], writes=[b_mean])
    kb.op("dve", lambda e: e.tensor_tensor(rstd[:], mean[:], mean[:], op=ALU.mult), reads=[b_mean], writes=[b_rstd])
    kb.op("dve", lambda e: e.scalar_tensor_tensor(rstd[:], pB[:], 1.0 / D, rstd[:], op0=ALU.mult, op1=ALU.subtract), reads=[bB, b_rstd], writes=[b_rstd])
    kb.op("dve", lambda e: e.tensor_scalar(rstd[:], rstd[:], LN_EPS, None, op0=ALU.add), reads=[b_rstd], writes=[b_rstd])
    kb.op("act", lambda e: e.activation(rstd[:], rstd[:], AF.Sqrt), reads=[b_rstd], writes=[b_rstd])
    kb.op("dve", lambda e: e.reciprocal(rstd[:], rstd[:]), reads=[b_rstd], writes=[b_rstd])
    for ch in range(NCH):
        k = ch % 2
        t1, b_t1 = ws["t1"][k], ws["b_t1"][k]
        kb.op("dve", lambda e, ch=ch, t1=t1: e.tensor_tensor(t1[:], v_aps[ch], mean[:], op=ALU.subtract), reads=[v_bufs[ch], b_mean], writes=[b_t1])
        kb.op("pool", lambda e, t1=t1: e.tensor_tensor(t1[:], t1[:], rstd[:], op=ALU.mult), reads=[b_t1, b_rstd], writes=[b_t1])
        kb.op("act", lambda e, ch=ch, t1=t1: e.activation(t1[:], t1[:], AF.Identity, scale=g_ap[:, ch:ch + 1], bias=b_ap[:, ch:ch + 1]),
              reads=[b_t1, gb_buf], writes=[b_t1])
        kb.dma("sp", out_dram[ch * 128:(ch + 1) * 128, tsl], t1[:], reads=[b_t1], writes=[out_buf])


def alloc_ln_ws(cx, st, pfx):
    ws = {}
    ws["sq"] = [cx.sb(st, f"{pfx}sq{k}", [128, 512]) for k in range(2)]
    ws["b_sq"] = [Buf() for _ in range(2)]
    ws["mean"] = cx.sb(st, f"{pfx}mean", [128, 512]); ws["b_mean"] = Buf()
    ws["rstd"] = cx.sb(st, f"{pfx}rstd", [128, 512]); ws["b_rstd"] = Buf()
    ws["t1"] = [cx.sb(st, f"{pfx}t1{k}", [128, 512]) for k in range(2)]
    ws["b_t1"] = [Buf() for _ in range(2)]
    return ws


PASS = 1024


def emit_moe_ln2(cx, c, dr, lay, h1T_dram, h1_buf, h2T_dram, h2_buf, n_exp=NE, n_pass=T // PASS, wl=0):
    kb, nc = cx.kb, cx.nc
    with ExitStack() as st:
        sb = lambda name, shape, dt=F32: cx.sb(st, name, shape, dt)
        acc = sb("acc", [128, NCH, PASS]); b_acc = [[Buf() for _ in range(2)] for _ in range(NCH)]
        xT = sb("xT", [128, NCH, PASS], BF16); b_xT = [Buf() for _ in range(NCH)]
        actT = sb("actT", [128, NCH, PASS], BF16); b_actT = [[Buf() for _ in range(2)] for _ in range(NCH)]
        stg = [sb(f"stg{k}", [128, NCH // 2, 256]) for k in range(2)]; b_stg = [Buf() for _ in range(2)]
        wbf = [sb(f"wbf{k}", [128, NCH, 256], BF16) for k in range(2)]; b_wbf = [Buf() for _ in range(2)]
        G = sb("G", [128, PASS]); b_G = [Buf() for _ in range(2)]
        tg = [sb(f"tg{k}", [128, 512]) for k in range(2)]; b_tg = [Buf() for _ in range(2)]
        tsg = [sb(f"tsg{k}", [128, 512]) for k in range(2)]; b_tsg = [Buf() for _ in range(2)]
        tu = [sb(f"tu{k}", [128, 512]) for k in range(2)]; b_tu = [Buf() for _ in range(2)]
        wr = sb("wr", [128, NCH, NE]); b_wr = Buf()
        rb = sb("rb", [128, NE]); b_rb = Buf()
        bg = sb("bg", [128, NE * NCH]); bu1 = sb("bu1", [128, NE * NCH]); b_bgu = Buf()
        lng = sb("lng", [128, NCH]); lnb = sb("lnb", [128, NCH]); b_lngb = Buf()
        gatesT = sb("gatesT", [32, PASS]); b_gatesT = Buf()
        gsel = sb("gsel", [32, PASS]); b_gsel = Buf()
        rt = {n: sb("rt_" + n, [128, NE]) for n in ("l", "e", "m", "em", "g")}
        rs = {n: sb("rs_" + n, [128, 8]) for n in ("m8", "nm1", "ss", "rs")}
        b_rt = Buf()
        ws = {"sq": tg, "b_sq": b_tg, "t1": tu, "b_t1": b_tu, "mean": tsg[0], "b_mean": b_tsg[0], "rstd": tsg[1], "b_rstd": b_tsg[1]}

        kb.dma("sp", wr[:], dr["router_w"][lay].rearrange("(c p) e -> p c e", p=128), writes=[b_wr])
        kb.dma("sp", rb[:], dr["router_b_bc"][lay], writes=[b_rb])
        kb.dma("sp", bg[:], dr["bg_fm"][lay], writes=[b_bgu])
        kb.dma("sp", bu1[:], dr["bu_fm"][lay], writes=[b_bgu])
        kb.op("pool", lambda e: e.tensor_scalar(bu1[:], bu1[:], 1.0, None, op0=ALU.add), reads=[b_bgu], writes=[b_bgu])
        kb.dma("sp", lng[:], dr["ln2_g_fm"][lay], writes=[b_lngb])
        kb.dma("sp", lnb[:], dr["ln2_b_fm"][lay], writes=[b_lngb])

        wgu = dr["w_gate_up"][wl]
        wdn = dr["w_down"][wl]
        cast_rr = [0]

        for ps_i in range(n_pass):
            t0 = ps_i * PASS
            for ch in range(NCH):
                kb.dma("sp", acc[:, ch, :], h1T_dram[ch * 128:(ch + 1) * 128, t0:t0 + PASS], reads=[h1_buf], writes=b_acc[ch])
            for ch in range(NCH):
                eng = ("pool", "act", "dve")[ch % 3]
                if eng == "act":
                    kb.op("act", lambda e, ch=ch: e.copy(xT[:, ch, :], acc[:, ch, :]), reads=b_acc[ch], writes=[b_xT[ch]])
                else:
                    kb.op(eng, lambda e, ch=ch: e.tensor_copy(xT[:, ch, :], acc[:, ch, :]), reads=b_acc[ch], writes=[b_xT[ch]])
            pR, bR = cx.psum[6]
            pT, bT = cx.psum[7]
            for tt in range(PASS // 128):
                for ch in range(NCH):
                    kb.op("pe", lambda e, ch=ch, tt=tt: e.matmul(pR[:, 0:NE], lhsT=acc[:, ch, tt * 128:(tt + 1) * 128], rhs=wr[:, ch, :],
                                                                 start=(ch == 0), stop=(ch == NCH - 1)),
                          reads=[b_acc[ch][tt // 4], b_wr], writes=[bR])
                kb.op("dve", lambda e: e.tensor_tensor(rt["l"][:], pR[:, 0:NE], rb[:], op=ALU.add), reads=[bR, b_rb], writes=[b_rt])
                kb.op("dve", lambda e: e.max(rs["m8"][:], rt["l"][:]), reads=[b_rt], writes=[b_rt])
                kb.op("dve", lambda e: e.tensor_scalar(rs["nm1"][:, 0:1], rs["m8"][:, 0:1], -1.0, None, op0=ALU.mult), reads=[b_rt], writes=[b_rt])
                kb.op("act", lambda e: e.activation(rt["e"][:], rt["l"][:], AF.Exp, bias=rs["nm1"][:, 0:1], scale=1.0), reads=[b_rt], writes=[b_rt])
                kb.op("dve", lambda e: e.tensor_scalar(rt["m"][:], rt["l"][:], rs["m8"][:, 3:4], None, op0=ALU.is_ge), reads=[b_rt], writes=[b_rt])
                kb.op("dve", lambda e: e.tensor_tensor(rt["em"][:], rt["e"][:], rt["m"][:], op=ALU.mult), reads=[b_rt], writes=[b_rt])
                kb.op("dve", lambda e: e.reduce_sum(rs["ss"][:, 0:1], rt["em"][:], axis=AX.X), reads=[b_rt], writes=[b_rt])
                kb.op("dve", lambda e: e.reciprocal(rs["rs"][:, 0:1], rs["ss"][:, 0:1]), reads=[b_rt], writes=[b_rt])
                kb.op("dve", lambda e: e.tensor_scalar(rt["g"][:], rt["em"][:], rs["rs"][:, 0:1], None, op0=ALU.mult), reads=[b_rt], writes=[b_rt])
                kb.op("pe", lambda e: e.transpose(pT[0:NE, 0:128], rt["g"][:], c["ident"][:]), reads=[b_rt, c["b_ident"]], writes=[bT])
                kb.op("act", lambda e, tt=tt: e.copy(gatesT[:, tt * 128:(tt + 1) * 128], pT[0:NE, 0:128]), reads=[bT], writes=[b_gatesT])
            for ch in range(NCH):
                for hf in range(2):
                    kb.op("pool", lambda e, ch=ch, hf=hf: e.tensor_scalar(acc[:, ch, hf * 512:(hf + 1) * 512], acc[:, ch, hf * 512:(hf + 1) * 512],
                                                                        DN_ALPHA, None, op0=ALU.mult),
                          reads=[], writes=[b_acc[ch][hf]])
            bdT = stg[0][0:32, :, :].rearrange("p a b -> p (a b)")
            kb.dma("sp", bdT, dr["b_down"][lay], writes=[b_stg[0]])
            for db in range(NCH):
                for tb in range(2):
                    pB_, bB_ = cx.psum[4 + (db * 2 + tb) % 2]
                    kb.op("pe", lambda e, db=db, tb=tb, pB_=pB_: e.matmul(pB_[:], lhsT=bdT[:, db * 128:(db + 1) * 128], rhs=gatesT[:, tb * 512:(tb + 1) * 512],
                                                                         start=True, stop=True),
                          reads=[b_stg[0], b_gatesT], writes=[bB_])
                    kb.op("dve", lambda e, db=db, tb=tb, pB_=pB_: e.tensor_tensor(acc[:, db, tb * 512:(tb + 1) * 512], acc[:, db, tb * 512:(tb + 1) * 512], pB_[:], op=ALU.add),
                          reads=[bB_], writes=[b_acc[db][tb]])

            pieces = []
            for ex in range(n_exp):
                for j in range(NCH):
                    pieces.append(("gu", ex, j))
                for dp in range(NCH // 2):
                    pieces.append(("dn", ex, dp))

            def issue_dma(i):
                kind, ex, j = pieces[i]
                k = i % 2
                src = wgu[ex] if kind == "gu" else wdn[ex]
                srcv = src[:, j * 256:(j + 1) * 256].rearrange("(c p) n -> p c n", p=128)
                h = NCH // 2
                kb.dma("sp", stg[0][:], srcv[:, 0:h, :], writes=[b_stg[0]])
                kb.dma("sp", stg[1][:], srcv[:, h:, :], writes=[b_stg[1]])

            def issue_cast(i):
                kind, ex, j = pieces[i]
                k = i % 2
                groups = [("pool", 0, 5), ("dve", 5, 8), ("dve", 8, 10), ("act", 10, 16)]
                for eng, a, b in groups:
                    hh = 0 if a < 8 else 1
                    a2, b2 = a - 8 * hh, b - 8 * hh
                    if kind == "gu":
                        src_ap = stg[hh][:, a2:b2, :].rearrange("p c (i two) -> p c two i", two=2)
                        dst_ap = wbf[k][:, a:b, :].rearrange("p c (two i) -> p c two i", two=2)
                    else:
                        src_ap = stg[hh][:, a2:b2, :]
                        dst_ap = wbf[k][:, a:b, :]
                    if eng == "act":
                        kb.op("act", lambda e, s=src_ap, d=dst_ap: e.copy(d, s), reads=[b_stg[hh]], writes=[b_wbf[k]])
                    else:
                        kb.op(eng, lambda e, s=src_ap, d=dst_ap: e.tensor_copy(d, s), reads=[b_stg[hh]], writes=[b_wbf[k]])

            def compute(i):
                kind, ex, j = pieces[i]
                k = i % 2
                if kind == "gu":
                    if j == 0:
                        kb.op("pool", lambda e: e.tensor_scalar(gsel[:], gatesT[:], c["ident"][0:32, ex:ex + 1], None, op0=ALU.mult),
                              reads=[b_gatesT, c["b_ident"]], writes=[b_gsel])
                        for tb in range(2):
                            pG, bG = cx.psum[6 + tb]
                            kb.op("pe", lambda e, tb=tb, pG=pG: e.matmul(pG[:], lhsT=c["ones"][0:32, :], rhs=gsel[:, tb * 512:(tb + 1) * 512], start=True, stop=True),
                                  reads=[c["b_ones"], b_gsel], writes=[bG])
                            kb.op("act", lambda e, tb=tb, pG=pG: e.copy(G[:, tb * 512:(tb + 1) * 512], pG[:]), reads=[bG], writes=[b_G[tb]])
                    for tb in range(2):
                        q = (j * 2 + tb) % 2
                        pg, bpg = cx.psum[q * 2]
                        pu, bpu = cx.psum[q * 2 + 1]
                        tsl = slice(tb * 512, (tb + 1) * 512)
                        for ch in range(NCH):
                            kb.op("pe", lambda e, ch=ch, pg=pg: e.matmul(pg[:], lhsT=wbf[k][:, ch, 0:128], rhs=xT[:, ch, tsl], start=(ch == 0), stop=(ch == NCH - 1)),
                                  reads=[b_wbf[k], b_xT[ch]], writes=[bpg])
                        for ch in range(NCH):
                            kb.op("pe", lambda e, ch=ch, pu=pu: e.matmul(pu[:], lhsT=wbf[k][:, ch, 128:256], rhs=xT[:, ch, tsl], start=(ch == 0), stop=(ch == NCH - 1)),
                                  reads=[b_wbf[k], b_xT[ch]], writes=[bpu])
                        col = ex * NCH + j
                        kb.op("dve", lambda e, pg=pg, q=q: e.tensor_scalar(tg[q][:], pg[:], bg[:, col:col + 1], 7.0, op0=ALU.add, op1=ALU.min),
                              reads=[bpg, b_bgu], writes=[b_tg[q]])
                        kb.op("act", lambda e, q=q: e.activation(tsg[q][:], tg[q][:], AF.Sigmoid, scale=1.702), reads=[b_tg[q]], writes=[b_tsg[q]])
                        kb.op("act", lambda e, pu=pu, q=q: e.activation(tu[q][:], pu[:], AF.Identity, bias=bu1[:, col:col + 1], scale=1.0),
                              reads=[bpu, b_bgu], writes=[b_tu[q]])
                        kb.op("pool", lambda e, q=q: e.tensor_scalar(tu[q][:], tu[q][:], 8.0, -6.0, op0=ALU.min, op1=ALU.max), reads=[b_tu[q]], writes=[b_tu[q]])
                        kb.op("pool", lambda e, q=q: e.tensor_tensor(tsg[q][:], tsg[q][:], tg[q][:], op=ALU.mult), reads=[b_tsg[q], b_tg[q]], writes=[b_tsg[q]])
                        kb.op("pool", lambda e, q=q: e.tensor_tensor(tu[q][:], tu[q][:], tsg[q][:], op=ALU.mult), reads=[b_tu[q], b_tsg[q]], writes=[b_tu[q]])
                        kb.op("dve", lambda e, q=q, tsl=tsl: e.tensor_tensor(actT[:, j, tsl], tu[q][:], G[:, tsl], op=ALU.mult),
                              reads=[b_tu[q], b_G[tb]], writes=[b_actT[j][tb]])
                else:
                    dp = j
                    for dbl in range(2):
                        db = dp * 2 + dbl
                        for tb in range(2):
                            pd, bpd = cx.psum[4 + (dbl * 2 + tb) % 2]
                            tsl = slice(tb * 512, (tb + 1) * 512)
                            for ch in range(NCH):
                                kb.op("pe", lambda e, ch=ch, pd=pd: e.matmul(pd[:], lhsT=wbf[k][:, ch, dbl * 128:(dbl + 1) * 128], rhs=actT[:, ch, tsl],
                                                                             start=(ch == 0), stop=(ch == NCH - 1)),
                                      reads=[b_wbf[k], b_actT[ch][tb]], writes=[bpd])
                            kb.op("dve", lambda e, pd=pd, db=db, tsl=tsl: e.tensor_tensor(acc[:, db, tsl], acc[:, db, tsl], pd[:], op=ALU.add),
                                  reads=[bpd], writes=[b_acc[db][tb]])

            n = len(pieces)
            if n > 0:
                issue_dma(0)
                issue_cast(0)
                if n > 1:
                    issue_dma(1)
                for i in range(n):
                    if i + 1 < n:
                        issue_cast(i + 1)
                    if i + 2 < n:
                        issue_dma(i + 2)
                    compute(i)

            for tb in range(2):
                tsl = slice(tb * 512, (tb + 1) * 512)
                v_aps = [acc[:, ch, tsl] for ch in range(NCH)]
                v_bufs = [b_acc[ch][tb] for ch in range(NCH)]
                emit_ln(cx, c, ws, v_aps, v_bufs, lng, lnb, b_lngb, h2T_dram, h2_buf, slice(t0 + tb * 512, t0 + (tb + 1) * 512), cx.psum[6], cx.psum[7])
        kb.barrier()


def fm(v):
    sh = v.shape
    return np.ascontiguousarray(v.reshape(sh[:-1] + (sh[-1] // 128, 128)).swapaxes(-1, -2))


def common_host_inputs(inputs, wl=None):
    h = {}
    h["ident"] = np.eye(128, dtype=np.float32)
    h["router_w"] = np.ascontiguousarray(inputs["router_w"])
    h["router_b_bc"] = np.ascontiguousarray(np.broadcast_to(inputs["router_b"][:, None, :], (2, 128, NE)))
    bgu = inputs["b_gate_up"]
    bgate = bgu[:, :, 0::2]
    bup = bgu[:, :, 1::2]
    h["bg_fm"] = np.ascontiguousarray(bgate.reshape(2, NE, NCH, 128).transpose(0, 3, 1, 2).reshape(2, 128, NE * NCH))
    h["bu_fm"] = np.ascontiguousarray(bup.reshape(2, NE, NCH, 128).transpose(0, 3, 1, 2).reshape(2, 128, NE * NCH))
    h["ln1_g_fm"] = fm(inputs["ln1_g"]); h["ln1_b_fm"] = fm(inputs["ln1_b"])
    h["ln2_g_fm"] = fm(inputs["ln2_g"]); h["ln2_b_fm"] = fm(inputs["ln2_b"])
    h["b_down"] = np.ascontiguousarray(inputs["b_down"])
    if wl is None:
        h["w_gate_up"] = inputs["w_gate_up"]
        h["w_down"] = inputs["w_down"]
    else:
        h["w_gate_up"] = inputs["w_gate_up"][wl:wl + 1]
        h["w_down"] = inputs["w_down"][wl:wl + 1]
    return h


def declare_inputs(nc, arrays):
    dr = {}
    for k, v in arrays.items():
        dt = {np.dtype(np.float32): F32}.get(v.dtype, None)
        if dt is None:
            dt = BF16
        dr[k] = nc.dram_tensor(k, list(v.shape), dt, kind="ExternalInput").ap()
    return dr


def emit_proj(cx, w_dram, cols, inT, b_inT, tblocks, epilogue, wk, ps_banks=(0, 1, 2, 3), post_block=None):
    kb = cx.kb
    stg, b_stg, wbf, b_wbf = wk
    n = len(cols)

    def issue_dma(i):
        k = i % 2
        srcv = w_dram[:, cols[i]:cols[i] + 128].rearrange("(c p) n -> p c n", p=128)
        kb.dma("sp", stg[k][:], srcv, writes=[b_stg[k]])

    def issue_cast(i):
        k = i % 2
        kb.op("pool", lambda e: e.tensor_copy(wbf[k][:, 0:6, :], stg[k][:, 0:6, :]), reads=[b_stg[k]], writes=[b_wbf[k]])
        kb.op("act", lambda e: e.copy(wbf[k][:, 6:11, :], stg[k][:, 6:11, :]), reads=[b_stg[k]], writes=[b_wbf[k]])
        kb.op("dve", lambda e: e.tensor_copy(wbf[k][:, 11:16, :], stg[k][:, 11:16, :]), reads=[b_stg[k]], writes=[b_wbf[k]])

    issue_dma(0)
    issue_cast(0)
    if n > 1:
        issue_dma(1)
    cnt = 0
    for i in range(n):
        k = i % 2
        if i + 1 < n:
            issue_cast(i + 1)
        if i + 2 < n:
            issue_dma(i + 2)
        for bi, (t0, tn) in enumerate(tblocks):
            p, bp = cx.psum[ps_banks[cnt % len(ps_banks)]]
            cnt += 1
            for ch in range(NCH):
                kb.op("pe", lambda e, ch=ch: e.matmul(p[:, 0:tn], lhsT=wbf[k][:, ch, :], rhs=inT[:, ch, t0:t0 + tn], start=(ch == 0), stop=(ch == NCH - 1)),
                      reads=[b_wbf[k], b_inT[ch]], writes=[bp])
            epilogue(i, bi, p, bp)
        if post_block is not None:
            post_block(i)


def alloc_wk(cx, st, pfx):
    stg = [cx.sb(st, f"{pfx}stg{k}", [128, NCH, 128]) for k in range(2)]
    wbf = [cx.sb(st, f"{pfx}wbf{k}", [128, NCH, 128], BF16) for k in range(2)]
    return stg, [Buf(), Buf()], wbf, [Buf(), Buf()]


def emit_mem_attn_head(cx, c, h, qT, b_qT, kT, vm, b_kv, out_ap_fn, b_out, tmp, ntok=T):
    kb = cx.kb
    ex, b_ex = tmp["ex"], tmp["b_ex"]
    rsum, b_rsum = tmp["rsum"], tmp["b_rsum"]
    for tb in range(ntok // 512):
        tsl = slice(tb * 512, (tb + 1) * 512)
        for mb in range(2):
            pS, bS = cx.psum[4 + mb]
            kb.op("pe", lambda e, mb=mb, pS=pS: e.matmul(pS[:], lhsT=kT[:, h, mb * 128:(mb + 1) * 128], rhs=qT[:, tsl], start=True, stop=True),
                  reads=[b_kv, b_qT], writes=[bS])
            kb.op("act", lambda e, mb=mb, pS=pS: e.activation(ex[mb][:], pS[:], AF.Exp, scale=128.0 ** -0.5), reads=[bS], writes=[b_ex[mb]])
        pO, bO = cx.psum[6]
        pZ, bZ = cx.psum[7]
        for mb in range(2):
            kb.op("pe", lambda e, mb=mb: e.matmul(pO[:], lhsT=vm[:, mb, h * 128:(h + 1) * 128], rhs=ex[mb][:], start=(mb == 0), stop=(mb == 1)),
                  reads=[b_kv, b_ex[mb]], writes=[bO])
        for mb in range(2):
            kb.op("pe", lambda e, mb=mb: e.matmul(pZ[:], lhsT=c["ones_bf"][:], rhs=ex[mb][:], start=(mb == 0), stop=(mb == 1)),
                  reads=[c["b_ones_bf"], b_ex[mb]], writes=[bZ])
        kb.op("dve", lambda e: e.reciprocal(rsum[:], pZ[:]), reads=[bZ], writes=[b_rsum])
        kb.op("dve", lambda e: e.tensor_tensor(out_ap_fn(tsl), pO[:], rsum[:], op=ALU.mult), reads=[bO, b_rsum], writes=[b_out])


def emit_mem_kv(cx, c, st, dr, lay):
    kb = cx.kb
    kT = cx.sb(st, "memkT", [128, 4, NMEM], BF16)
    vm = cx.sb(st, "memv", [128, 2, MEM_W], BF16)
    b_kv = Buf()
    with ExitStack() as s2:
        memT = cx.sb(s2, "memT", [128, NCH, NMEM], BF16); b_memT = [Buf() for _ in range(NCH)]
        mtok = cx.sb(s2, "mtok", [128, D]); b_mtok = Buf()
        wk = alloc_wk(cx, s2, "mk")
        for mb in range(2):
            kb.dma("sp", mtok[:], dr["mem"][mb * 128:(mb + 1) * 128, :], writes=[b_mtok])
            for ch in range(NCH):
                p, bp = cx.psum[ch % 2]
                kb.op("pe", lambda e, ch=ch, p=p: e.transpose(p[:, 0:128], mtok[:, ch * 128:(ch + 1) * 128], c["ident"][:]), reads=[b_mtok, c["b_ident"]], writes=[bp])
                kb.op("dve", lambda e, ch=ch, p=p, mb=mb: e.tensor_copy(memT[:, ch, mb * 128:(mb + 1) * 128], p[:, 0:128]), reads=[bp], writes=[b_memT[ch]])
        wkv = dr["mem_w_kv"][lay]
        def epi_k(i, bi, p, bp):
            kb.op("act", lambda e: e.copy(kT[:, i, :], p[:, 0:NMEM]), reads=[bp], writes=[b_kv])
        emit_proj(cx, wkv, [hh * 128 for hh in range(4)], memT, b_memT, [(0, NMEM)], epi_k, wk)
        stg, b_stg, wbf, b_wbf = wk
        for nb in range(4):
            k = nb % 2
            kb.dma("sp", stg[k][:], wkv[:, MEM_W + nb * 128:MEM_W + (nb + 1) * 128].rearrange("(c p) n -> p c n", p=128), writes=[b_stg[k]])
            kb.op("dve", lambda e, k=k: e.tensor_copy(wbf[k][:], stg[k][:]), reads=[b_stg[k]], writes=[b_wbf[k]])
            for mb in range(2):
                p, bp = cx.psum[2 + mb]
                for ch in range(NCH):
                    kb.op("pe", lambda e, ch=ch, p=p, mb=mb, k=k: e.matmul(p[:, 0:128], lhsT=memT[:, ch, mb * 128:(mb + 1) * 128], rhs=wbf[k][:, ch, :],
                                                                           start=(ch == 0), stop=(ch == NCH - 1)),
                          reads=[b_memT[ch], b_wbf[k]], writes=[bp])
                kb.op("act", lambda e, p=p, mb=mb, nb=nb: e.copy(vm[:, mb, nb * 128:(nb + 1) * 128], p[:, 0:128]), reads=[bp], writes=[b_kv])
        kb.barrier()
    return kT, vm, b_kv


def emit_load_transpose(cx, c, st_tmp, src_rows, nrows, inT, b_inT, col0, hT_dram=None, hT_buf=None, hT_col0=0):
    kb = cx.kb
    rows = [cx.sb(st_tmp, f"ltrow{k}", [128, D]) for k in range(2)]; b_rows = [Buf(), Buf()]
    f32t = [cx.sb(st_tmp, f"ltf{k}", [128, 128]) for k in range(2)]; b_f = [Buf(), Buf()]
    nt = (nrows + 127) // 128
    cnt = 0
    for ti in range(nt):
        r0 = ti * 128
        rn = min(128, nrows - r0)
        k = ti % 2
        kb.dma("sp", rows[k][0:rn, :], src_rows[r0:r0 + rn, :], writes=[b_rows[k]])
        for ch in range(NCH):
            p, bp = cx.psum[cnt % 4]
            kb.op("pe", lambda e, ch=ch, p=p: e.transpose(p[:, 0:rn], rows[k][0:rn, ch * 128:(ch + 1) * 128], c["ident"][0:rn, 0:rn]),
                  reads=[b_rows[k], c["b_ident"]], writes=[bp])
            kb.op("act", lambda e, ch=ch, p=p: e.copy(inT[:, ch, col0 + r0:col0 + r0 + rn], p[:, 0:rn]), reads=[bp], writes=[b_inT[ch]])
            if hT_dram is not None:
                q = cnt % 2
                kb.op("dve", lambda e, p=p, q=q: e.tensor_copy(f32t[q][:, 0:rn], p[:, 0:rn]), reads=[bp], writes=[b_f[q]])
                kb.dma("sp", hT_dram[ch * 128:(ch + 1) * 128, hT_col0 + r0:hT_col0 + r0 + rn], f32t[q][:, 0:rn], reads=[b_f[q]], writes=[hT_buf])
            cnt += 1


def emit_load_fm_bf16(cx, st_tmp, hT_dram, hT_buf, inT, b_inT, ntok=T):
    kb = cx.kb
    tmpf = [cx.sb(st_tmp, f"lfm{k}", [128, ntok]) for k in range(2)]; b_t = [Buf(), Buf()]
    for ch in range(NCH):
        k = ch % 2
        kb.dma("sp", tmpf[k][:], hT_dram[ch * 128:(ch + 1) * 128, 0:ntok], reads=[hT_buf], writes=[b_t[k]])
        eng = ("pool", "dve")[ch % 2]
        kb.op(eng, lambda e, ch=ch, k=k: e.tensor_copy(inT[:, ch, 0:ntok], tmpf[k][:]), reads=[b_t[k]], writes=[b_inT[ch]])


def emit_wout_ln1(cx, c, dr, lay, w_out_dram, mix_dram, mix_buf, hT_in, hin_buf, h1T, h1_buf):
    kb = cx.kb
    with ExitStack() as s2:
        mixT = cx.sb(s2, "mixT", [128, NCH, T], BF16); b_mixT = [Buf() for _ in range(NCH)]
        for ch in range(NCH):
            kb.dma("sp", mixT[:, ch, :], mix_dram[ch * 128:(ch + 1) * 128, :], reads=[mix_buf], writes=[b_mixT[ch]])
        wk = alloc_wk(cx, s2, "wo")
        v = cx.sb(s2, "v_ln1", [128, NCH, 512]); b_v = [Buf() for _ in range(NCH)]
        res = [cx.sb(s2, f"res{k}", [128, 512]) for k in range(2)]; b_res = [Buf(), Buf()]
        lng = cx.sb(s2, "ln1g", [128, NCH]); lnb = cx.sb(s2, "ln1b", [128, NCH]); b_gb = Buf()
        kb.dma("sp", lng[:], dr["ln1_g_fm"][lay], writes=[b_gb])
        kb.dma("sp", lnb[:], dr["ln1_b_fm"][lay], writes=[b_gb])
        ws = alloc_ln_ws(cx, s2, "ln1")
        for tb in range(T // 512):
            tsl = slice(tb * 512, (tb + 1) * 512)

            def epi(i, bi, p, bp):
                k = i % 2
                kb.dma("sp", res[k][:], hT_in[i * 128:(i + 1) * 128, tsl], reads=[hin_buf], writes=[b_res[k]])
                kb.op("dve", lambda e: e.scalar_tensor_tensor(v[:, i, :], res[k][:], DN_ALPHA, p[:], op0=ALU.mult, op1=ALU.add),
                      reads=[b_res[k], bp], writes=[b_v[i]])
            emit_proj(cx, w_out_dram, [i * 128 for i in range(NCH)], mixT, b_mixT, [(tb * 512, 512)], epi, wk)
            emit_ln(cx, c, ws, [v[:, ch, :] for ch in range(NCH)], b_v, lng, lnb, b_gb, h1T, h1_buf, tsl, cx.psum[6], cx.psum[7])
        kb.barrier()


def emit_lru_params(cx, c, st, dr):
    kb = cx.kb
    P = {}
    b = Buf()
    for name in ("cw0", "cw1", "cw2", "cw3", "cb", "rgb", "igb", "lam"):
        P[name] = cx.sb(st, "lp_" + name, [128, 12])
        kb.dma("sp", P[name][:], dr["lru_" + name], writes=[b])
    t = {n: cx.sb(st, "lp_t" + n, [128, 12]) for n in ("a", "y", "z", "z2", "s")}
    lam = P["lam"]
    kb.op("dve", lambda e: e.tensor_scalar(t["a"][:], lam[:], -1.0, None, op0=ALU.mult), reads=[b], writes=[b])
    kb.op("dve", lambda e: e.tensor_tensor(t["y"][:], lam[:], t["a"][:], op=ALU.max), reads=[b], writes=[b])
    kb.op("act", lambda e: e.activation(t["y"][:], t["y"][:], AF.Exp, scale=-1.0), reads=[b], writes=[b])
    kb.op("dve", lambda e: e.tensor_scalar(t["z"][:], t["y"][:], 2.0, None, op0=ALU.add), reads=[b], writes=[b])
    kb.op("dve", lambda e: e.reciprocal(t["z"][:], t["z"][:]), reads=[b], writes=[b])
    kb.op("dve", lambda e: e.tensor_tensor(t["z"][:], t["z"][:], t["y"][:], op=ALU.mult), reads=[b], writes=[b])
    kb.op("dve", lambda e: e.tensor_tensor(t["z2"][:], t["z"][:], t["z"][:], op=ALU.mult), reads=[b], writes=[b])
    kb.op("dve", lambda e: e.tensor_scalar(t["s"][:], t["z2"][:], 1.0 / 17, 1.0 / 15, op0=ALU.mult, op1=ALU.add), reads=[b], writes=[b])
    for kk in (13, 11, 9, 7, 5, 3, 1):
        kb.op("dve", lambda e: e.tensor_tensor(t["s"][:], t["s"][:], t["z2"][:], op=ALU.mult), reads=[b], writes=[b])
        kb.op("dve", lambda e, kk=kk: e.tensor_scalar(t["s"][:], t["s"][:], 1.0 / kk, None, op0=ALU.add), reads=[b], writes=[b])
    kb.op("dve", lambda e: e.tensor_tensor(t["s"][:], t["s"][:], t["z"][:], op=ALU.mult), reads=[b], writes=[b])
    kb.op("dve", lambda e: e.tensor_scalar(t["s"][:], t["s"][:], 2.0, None, op0=ALU.mult), reads=[b], writes=[b])
    kb.op("dve", lambda e: e.tensor_scalar(t["a"][:], t["a"][:], 0.0, None, op0=ALU.max), reads=[b], writes=[b])
    kb.op("dve", lambda e: e.tensor_tensor(t["s"][:], t["s"][:], t["a"][:], op=ALU.add), reads=[b], writes=[b])
    P["nsc"] = cx.sb(st, "lp_nsc", [128, 12])
    kb.op("dve", lambda e: e.tensor_scalar(P["nsc"][:], t["s"][:], -8.0, None, op0=ALU.mult), reads=[b], writes=[b])
    P["buf"] = b
    return P


HALO = 8


def emit_l0_mixer(cx, c, dr, xT, b_xT, mix_dram, mix_buf, hin_ap, hin_buf, summ_dram=None, summ_buf=None, memkv=None):
    kb = cx.kb
    summary = summ_dram is not None
    w_in = dr["a_w_in"]
    TT = HALO + T
    with ExitStack() as st:
        P = emit_lru_params(cx, c, st, dr)
        pb = P["buf"]
        wk = alloc_wk(cx, st, "l0")
        rgw = [cx.sb(st, f"rgw{k}", [128, 128]) for k in range(2)]
        igw = [cx.sb(st, f"igw{k}", [128, 128]) for k in range(2)]
        b_gw = [Buf(), Buf()]
        u = cx.sb(st, "lru_u", [128, TT]); b_u = Buf()
        xc = cx.sb(st, "lru_xc", [128, T]); b_xc = Buf()
        ra = cx.sb(st, "lru_ra", [128, T]); b_ra = Buf()
        ii = cx.sb(st, "lru_i", [128, T]); b_ii = Buf()
        bb = cx.sb(st, "lru_b", [128, T]); b_bb = Buf()
        gl = cx.sb(st, "lru_gl", [128, T]); b_gl = Buf()
        g2 = cx.sb(st, "lru_g2", [128, T]); b_g2 = Buf()
        mo = [cx.sb(st, f"lru_mo{k}", [128, T], BF16) for k in range(2)]; b_mo = [Buf(), Buf()]
        sm = cx.sb(st, "lru_sm", [128, 12, 2]); b_sm = Buf()
        racc = cx.sb(st, "lru_racc", [128, 4]); b_racc = Buf()
        if summary:
            kb.op("pool", lambda e: e.memset(sm[:], 0.0), writes=[b_sm])
        tblocks = [(0, HALO)] + [(HALO + i * 512, 512) for i in range(T // 512)]

        for n in range(12):
            k = n % 2
            kb.dma("sp", rgw[k][:], dr["a_rg_w"][n], writes=[b_gw[k]])
            kb.dma("sp", igw[k][:], dr["a_ig_w"][n], writes=[b_gw[k]])
            cols = [LRU_W + n * 128] if summary else [LRU_W + n * 128, n * 128]

            def epi(i, bi, p, bp):
                t0, tn = tblocks[bi]
                if i == 0:
                    kb.op("act", lambda e: e.copy(u[:, t0:t0 + tn], p[:, 0:tn]), reads=[bp], writes=[b_u])
                else:
                    if bi == 0:
                        return
                    o0 = t0 - HALO
                    kb.op("act", lambda e: e.copy(gl[:, o0:o0 + tn], p[:, 0:tn]), reads=[bp], writes=[b_gl])
            emit_proj(cx, w_in, cols, xT, b_xT, tblocks, epi, wk)

            kb.op("dve", lambda e: e.tensor_scalar(xc[:], u[:, HALO:HALO + T], P["cw3"][:, n:n + 1], P["cb"][:, n:n + 1], op0=ALU.mult, op1=ALU.add),
                  reads=[b_u, pb], writes=[b_xc])
            for tap, nm in ((1, "cw2"), (2, "cw1"), (3, "cw0")):
                kb.op("dve", lambda e, tap=tap, nm=nm: e.scalar_tensor_tensor(xc[:], u[:, HALO - tap:HALO - tap + T], P[nm][:, n:n + 1], xc[:], op0=ALU.mult, op1=ALU.add),
                      reads=[b_u, pb, b_xc], writes=[b_xc])
            for tb in range(T // 512):
                tsl = slice(tb * 512, (tb + 1) * 512)
                pr, bpr = cx.psum[4 + tb % 2]
                pi, bpi = cx.psum[6 + tb % 2]
                kb.op("pe", lambda e, pr=pr: e.matmul(pr[:], lhsT=rgw[k][:], rhs=xc[:, tsl], start=True, stop=True), reads=[b_gw[k], b_xc], writes=[bpr])
                kb.op("pe", lambda e, pi=pi: e.matmul(pi[:], lhsT=igw[k][:], rhs=xc[:, tsl], start=True, stop=True), reads=[b_gw[k], b_xc], writes=[bpi])
                if summary:
                    kb.op("act", lambda e, pr=pr, tb=tb: e.activation(ra[:, tsl], pr[:], AF.Sigmoid, bias=P["rgb"][:, n:n + 1], scale=1.0, accum_out=racc[:, tb:tb + 1]),
                          reads=[bpr, pb], writes=[b_ra, b_racc])
                else:
                    kb.op("act", lambda e, pr=pr: e.activation(ra[:, tsl], pr[:], AF.Sigmoid, bias=P["rgb"][:, n:n + 1], scale=1.0), reads=[bpr, pb], writes=[b_ra])
                kb.op("act", lambda e, pi=pi: e.activation(ii[:, tsl], pi[:], AF.Sigmoid, bias=P["igb"][:, n:n + 1], scale=1.0), reads=[bpi, pb], writes=[b_ii])
            kb.op("act", lambda e: e.activation(ra[:], ra[:], AF.Exp, scale=P["nsc"][:, n:n + 1]), reads=[b_ra, pb], writes=[b_ra])
            kb.op("pool", lambda e: e.tensor_tensor(bb[:], ra[:], ra[:], op=ALU.mult), reads=[b_ra], writes=[b_bb])
            kb.op("pool", lambda e: e.tensor_scalar(bb[:], bb[:], -1.0, 1.0, op0=ALU.mult, op1=ALU.add), reads=[b_bb], writes=[b_bb])
            kb.op("act", lambda e: e.activation(bb[:], bb[:], AF.Sqrt), reads=[b_bb], writes=[b_bb])
            kb.op("pool", lambda e: e.tensor_tensor(ii[:], ii[:], xc[:], op=ALU.mult), reads=[b_ii, b_xc], writes=[b_ii])
            kb.op("pool", lambda e: e.tensor_tensor(bb[:], bb[:], ii[:], op=ALU.mult), reads=[b_bb, b_ii], writes=[b_bb])
            if summary:
                kb.op("dve", lambda e: e.tensor_tensor_scan(xc[:], ra[:], bb[:], 0.0, op0=ALU.mult, op1=ALU.add), reads=[b_ra, b_bb], writes=[b_xc])
                kb.op("dve", lambda e: e.tensor_copy(sm[:, n, 0:1], xc[:, T - 1:T]), reads=[b_xc], writes=[b_sm])
                kb.op("dve", lambda e: e.reduce_sum(sm[:, n, 1:2], racc[:, 0:4], axis=AX.X), reads=[b_racc], writes=[b_sm])
                continue
            kb.op("dve", lambda e: e.tensor_tensor_scan(xc[:], ra[:], bb[:], hin_ap[:, n:n + 1], op0=ALU.mult, op1=ALU.add), reads=[b_ra, b_bb, hin_buf], writes=[b_xc])
            kb.op("pool", lambda e: e.tensor_tensor(g2[:], gl[:], gl[:], op=ALU.mult), reads=[b_gl], writes=[b_g2])
            kb.op("pool", lambda e: e.tensor_scalar(g2[:], g2[:], 0.044715, 1.0, op0=ALU.mult, op1=ALU.add), reads=[b_g2], writes=[b_g2])
            kb.op("pool", lambda e: e.tensor_tensor(g2[:], g2[:], gl[:], op=ALU.mult), reads=[b_g2, b_gl], writes=[b_g2])
            kb.op("act", lambda e: e.activation(g2[:], g2[:], AF.Sigmoid, scale=1.5957691216057308), reads=[b_g2], writes=[b_g2])
            kb.op("pool", lambda e: e.tensor_tensor(g2[:], g2[:], gl[:], op=ALU.mult), reads=[b_g2, b_gl], writes=[b_g2])
            kb.op("dve", lambda e: e.tensor_tensor(mo[k][:], xc[:], g2[:], op=ALU.mult), reads=[b_xc, b_g2], writes=[b_mo[k]])
            kb.dma("sp", mix_dram[n * 128:(n + 1) * 128, :], mo[k][:], reads=[b_mo[k]], writes=[mix_buf])
        if summary:
            kb.dma("sp", summ_dram, sm[:], reads=[b_sm], writes=[summ_buf])
            kb.barrier()
            return
        kT, vm, b_kv = memkv
        qT = [cx.sb(st, f"qmT{k}", [128, T], BF16) for k in range(2)]; b_qT = [Buf(), Buf()]
        tmp = {"ex": [cx.sb(st, f"mex{k}", [128, 512], BF16) for k in range(2)], "b_ex": [Buf(), Buf()],
               "rsum": cx.sb(st, "mrs", [128, 512]), "b_rsum": Buf()}
        for h in range(4):
            k = h % 2

            def epi_q(i, bi, p, bp):
                t0, tn = tblocks[1 + bi]
                kb.op("act", lambda e: e.copy(qT[k][:, t0 - HALO:t0 - HALO + tn], p[:, 0:tn]), reads=[bp], writes=[b_qT[k]])
            emit_proj(cx, w_in, [2 * LRU_W + h * 128], xT, b_xT, tblocks[1:], epi_q, wk)
            emit_mem_attn_head(cx, c, h, qT[k], b_qT[k], kT, vm, b_kv, lambda tsl, k=k: mo[k][:, tsl], b_mo[k], tmp)
            kb.dma("sp", mix_dram[(12 + h) * 128:(13 + h) * 128, :], mo[k][:], reads=[b_mo[k]], writes=[mix_buf])
        kb.barrier()


def emit_carry(cx, c, st, dr, summ_src=None, summ_buf=None, lam_p=None):
    kb = cx.kb
    b = Buf()
    sa = cx.sb(st, "cy_sa", [128, NCORES, 12, 2])
    sel = cx.sb(st, "cy_sel", [128, NCORES])
    if lam_p is None:
        lam_p = emit_lru_params(cx, c, st, dr)
    pb = lam_p["buf"]
    if summ_src is None:
        kb.dma("sp", sa[:], dr["summ_all"].rearrange("k p n two -> p k n two"), writes=[b])
    else:
        kb.dma("sp", sa[:], summ_src.rearrange("(k p) (n two) -> p k n two", p=128, two=2), reads=[summ_buf], writes=[b])
    kb.dma("sp", sel[:], dr["core_sel"], writes=[b])
    H = cx.sb(st, "cy_H", [128, 12]); hin = cx.sb(st, "cy_hin", [128, 12]); Pk = cx.sb(st, "cy_P", [128, 12]); tmp = cx.sb(st, "cy_t", [128, 12])
    kb.op("dve", lambda e: e.memset(H[:], 0.0), writes=[b])
    kb.op("dve", lambda e: e.memset(hin[:], 0.0), reads=[b], writes=[b])
    for k in range(NCORES):
        kb.op("dve", lambda e, k=k: e.scalar_tensor_tensor(hin[:], H[:], sel[:, k:k + 1], hin[:], op0=ALU.mult, op1=ALU.add), reads=[b], writes=[b])
        if k == NCORES - 1:
            break
        kb.op("dve", lambda e, k=k: e.tensor_tensor(Pk[:], sa[:, k, :, 1], lam_p["nsc"][:], op=ALU.mult), reads=[b, pb], writes=[b])
        kb.op("act", lambda e: e.activation(Pk[:], Pk[:], AF.Exp), reads=[b], writes=[b])
        kb.op("dve", lambda e: e.tensor_tensor(tmp[:], Pk[:], H[:], op=ALU.mult), reads=[b], writes=[b])
        kb.op("dve", lambda e, k=k: e.tensor_tensor(H[:], tmp[:], sa[:, k, :, 0], op=ALU.add), reads=[b], writes=[b])
    return hin, b


def lru_host_inputs(inputs):
    h = {}
    cw = inputs["a_conv_w"][0]
    for i in range(4):
        h[f"lru_cw{i}"] = fm(cw[i])
    h["lru_cb"] = fm(inputs["a_conv_b"][0]); h["lru_rgb"] = fm(inputs["a_rg_b"][0]); h["lru_igb"] = fm(inputs["a_ig_b"][0])
    h["lru_lam"] = fm(inputs["a_lambda"][0])
    h["a_w_in"] = inputs["a_w_in"][0]
    h["a_rg_w"] = inputs["a_rg_w"][0]; h["a_ig_w"] = inputs["a_ig_w"][0]
    return h


def x_ext_for_core(x, cidx):
    xe = np.zeros((HALO + T, D), np.float32)
    xe[HALO:] = x[cidx * T:(cidx + 1) * T]
    if cidx > 0:
        xe[:HALO] = x[cidx * T - HALO:cidx * T]
    return xe


def build_A(arrays):
    nc = bass.Bass("TRN2", target_bir_lowering=False)
    dr = declare_inputs(nc, arrays)
    summ = nc.dram_tensor("summ", [128, 12, 2], F32, kind="ExternalOutput").ap()
    with ExitStack() as st:
        kb = KB(nc, st); cx = Ctx(nc, kb, st)
        c = load_consts(cx, st, dr)
        xT = cx.sb(st, "xT", [128, NCH, HALO + T], BF16); b_xT = [Buf() for _ in range(NCH)]
        with ExitStack() as s2:
            emit_load_transpose(cx, c, s2, dr["x_ext"], HALO + T, xT, b_xT, 0)
            kb.barrier()
        emit_l0_mixer(cx, c, dr, xT, b_xT, None, None, None, None, summ_dram=summ, summ_buf=Buf())
        kb.barrier()
    return nc


def build_B(arrays, upto="all", n_exp=NE):
    nc = bass.Bass("TRN2", target_bir_lowering=False)
    dr = declare_inputs(nc, arrays)
    outs = {}
    h2T = nc.dram_tensor("h2T", [D, T], F32, kind="ExternalOutput").ap()
    mix_dram = nc.dram_tensor("mix_dram", [D, T], BF16, kind="ExternalOutput" if upto == "mixer" else "Internal").ap()
    hT0 = nc.dram_tensor("hT0", [D, HALO + T], F32, kind="Internal").ap()
    h1T = nc.dram_tensor("h1T", [D, T], F32, kind="ExternalOutput" if upto == "ln1" else "Internal").ap()
    if upto == "all":
        kT_out = nc.dram_tensor("kT_out", [SB_H * 128, T], BF16, kind="ExternalOutput").ap()
        v_out = nc.dram_tensor("v_out", [T, SB_H * 128], BF16, kind="ExternalOutput").ap()
    with ExitStack() as st:
        kb = KB(nc, st); cx = Ctx(nc, kb, st)
        c = load_consts(cx, st, dr)
        b_hT0 = Buf(); b_mix = Buf(); b_h1 = Buf(); b_h2 = Buf()
        with ExitStack() as sm:
            xT = cx.sb(sm, "xT", [128, NCH, HALO + T], BF16); b_xT = [Buf() for _ in range(NCH)]
            with ExitStack() as s2:
                emit_load_transpose(cx, c, s2, dr["x_ext"], HALO + T, xT, b_xT, 0, hT_dram=hT0, hT_buf=b_hT0)
                kb.barrier()
            hin, b_hin = emit_carry(cx, c, sm, dr)
            memkv = emit_mem_kv(cx, c, sm, dr, 0)
            emit_l0_mixer(cx, c, dr, xT, b_xT, mix_dram, b_mix, hin, b_hin, memkv=memkv)
            kb.barrier()
        if upto != "mixer":
            emit_wout_ln1(cx, c, dr, 0, dr["a_w_out"], mix_dram, b_mix, hT0[:, HALO:], b_hT0, h1T, b_h1)
        if upto == "all":
            emit_moe_ln2(cx, c, dr, 0, h1T, b_h1, h2T, b_h2, n_exp=n_exp)
            emit_kv_proj(cx, c, dr, h2T, b_h2, kT_out, v_out, Buf())
        kb.barrier()
    return nc


SB_H = 12
NKB = S // 128


def emit_kv_proj(cx, c, dr, h2T, b_h2, kT_out, v_out, b_out):
    kb = cx.kb
    wkv = dr["w_kv_shared"]
    with ExitStack() as st:
        inT = cx.sb(st, "kvinT", [128, NCH, T], BF16); b_inT = [Buf() for _ in range(NCH)]
        with ExitStack() as s2:
            emit_load_fm_bf16(cx, s2, h2T, b_h2, inT, b_inT)
            kb.barrier()
        wk = alloc_wk(cx, st, "kv")
        ko = [cx.sb(st, f"kvo{k}", [128, 512], BF16) for k in range(2)]; b_ko = [Buf(), Buf()]
        cnt = [0]

        def epi(i, bi, p, bp):
            k = cnt[0] % 2; cnt[0] += 1
            kb.op("act", lambda e: e.copy(ko[k][:], p[:]), reads=[bp], writes=[b_ko[k]])
            kb.dma("sp", kT_out[i * 128:(i + 1) * 128, bi * 512:(bi + 1) * 512], ko[k][:], reads=[b_ko[k]], writes=[b_out])
        emit_proj(cx, wkv, [i * 128 for i in range(SB_H)], inT, b_inT, [(i * 512, 512) for i in range(T // 512)], epi, wk)
        stg, b_stg, wbf, b_wbf = wk
        for nb in range(SB_H):
            k = nb % 2
            kb.dma("sp", stg[k][:], wkv[:, 1536 + nb * 128:1536 + (nb + 1) * 128].rearrange("(c p) n -> p c n", p=128), writes=[b_stg[k]])
            kb.op("dve", lambda e, k=k: e.tensor_copy(wbf[k][:, 0:8, :], stg[k][:, 0:8, :]), reads=[b_stg[k]], writes=[b_wbf[k]])
            kb.op("pool", lambda e, k=k: e.tensor_copy(wbf[k][:, 8:16, :], stg[k][:, 8:16, :]), reads=[b_stg[k]], writes=[b_wbf[k]])
            for tg4 in range(T // 512):
                p, bp = cx.psum[tg4 % 4]
                for tt in range(4):
                    t0 = tg4 * 512 + tt * 128
                    for ch in range(NCH):
                        kb.op("pe", lambda e, ch=ch, p=p, tt=tt, t0=t0, k=k: e.matmul(p[:, tt * 128:(tt + 1) * 128], lhsT=inT[:, ch, t0:t0 + 128], rhs=wbf[k][:, ch, :],
                                                                                      start=(ch == 0), stop=(ch == NCH - 1)),
                              reads=[b_inT[ch], b_wbf[k]], writes=[bp])
                kk = cnt[0] % 2; cnt[0] += 1
                kb.op("act", lambda e, p=p, kk=kk: e.copy(ko[kk][:], p[:]), reads=[bp], writes=[b_ko[kk]])
                kb.dma("sp", v_out[tg4 * 512:(tg4 + 1) * 512, nb * 128:(nb + 1) * 128].rearrange("(a p) n -> p a n", p=128),
                       ko[kk][:].rearrange("p (a n) -> p a n", a=4), reads=[b_ko[kk]], writes=[b_out])
        kb.barrier()


def emit_l1_mixer(cx, c, dr, inT, b_inT, mix_dram, mix_buf, memkv, n_heads=SB_H, n_kb=NKB, kv_src=None, kv_buf=None):
    kb = cx.kb
    wq = dr["b_w_q"]
    SEG = 2048
    if kv_src is None:
        kT_all, v_all = dr["kT_all"], dr["v_all"]
        kslice = lambda h, seg: kT_all[h * 128:(h + 1) * 128, seg * SEG:(seg + 1) * SEG]
        kv_reads = []
    else:
        kT_g, v_all = kv_src
        kslice = lambda h, seg: kT_g[seg * 1536 + h * 128:seg * 1536 + (h + 1) * 128, :]
        kv_reads = [kv_buf]
    with ExitStack() as st:
        wk = alloc_wk(cx, st, "l1")
        qT = [cx.sb(st, f"sbq{k}", [128, T], BF16) for k in range(2)]; b_qT = [Buf(), Buf()]
        mo = [cx.sb(st, f"sbmo{k}", [128, T], BF16) for k in range(2)]; b_mo = [Buf(), Buf()]
        kseg = [cx.sb(st, f"kseg{k}", [128, SEG], BF16) for k in range(2)]; b_kseg = [Buf(), Buf()]
        vseg = [cx.sb(st, f"vseg{k}", [128, SEG // 128, 128], BF16) for k in range(2)]; b_vseg = [Buf(), Buf()]
        qpos = cx.sb(st, "qpos", [128, 4, 512]); kpos = cx.sb(st, "kpos", [128, NKB]); b_pos = Buf()
        negU = cx.sb(st, "negU", [128, 128], BF16); negUf = cx.sb(st, "negUf", [128, 128]); b_negU = Buf()
        negones = cx.sb(st, "negones", [128, 128]); b_negones = Buf()
        kb.dma("sp", qpos[:], dr["qpos"], writes=[b_pos])
        kb.dma("sp", kpos[:], dr["kpos"], writes=[b_pos])
        kb.dma("sp", negUf[:], dr["negU"], writes=[b_negU])
        kb.op("dve", lambda e: e.tensor_copy(negU[:], negUf[:]), reads=[b_negU], writes=[b_negU])
        kb.op("pool", lambda e: e.memset(negones[:], -1.0), writes=[b_negones])
        NT = 3
        te = [cx.sb(st, f"sb_e{k}", [128, 512]) for k in range(NT)]; b_te = [Buf() for _ in range(NT)]
        tsm = [cx.sb(st, f"sb_sm{k}", [128, 512]) for k in range(NT)]; b_tsm = [Buf() for _ in range(NT)]
        tsb = [cx.sb(st, f"sb_sb{k}", [128, 512], BF16) for k in range(NT)]; b_tsb = [Buf() for _ in range(NT)]
        tw = [cx.sb(st, f"sb_w{k}", [128, 512], BF16) for k in range(NT)]; b_tw = [Buf() for _ in range(NT)]
        spacc = cx.sb(st, "sb_acc", [128, 512]); b_spacc = Buf()
        pair = 0
        tblocks = [(i * 512, 512) for i in range(T // 512)]
        for h in range(n_heads):
            k2 = h % 2

            def epi_q(i, bi, p, bp):
                kb.op("act", lambda e: e.mul(qT[k2][:, bi * 512:(bi + 1) * 512], p[:], 128.0 ** -0.5), reads=[bp], writes=[b_qT[k2]])
            emit_proj(cx, wq, [h * 128], inT, b_inT, tblocks, epi_q, wk, ps_banks=(6, 7))
            for Q in range(T // 512):
                qsl = slice(Q * 512, (Q + 1) * 512)
                pO, bO = cx.psum[4 + Q % 2]
                first = True
                for seg in range(n_kb * 128 // SEG - 1, -1, -1):
                    ks = seg % 2
                    kb.dma("sp", kseg[ks][:], kslice(h, seg), reads=kv_reads, writes=[b_kseg[ks]])
                    kb.dma("sp", vseg[ks][:], v_all[seg * SEG:(seg + 1) * SEG, h * 128:(h + 1) * 128].rearrange("(a p) n -> p a n", p=128), reads=kv_reads, writes=[b_vseg[ks]])
                    for bl in range(SEG // 128 - 1, -1, -1):
                        B = seg * (SEG // 128) + bl
                        i3 = pair % NT; pair += 1
                        pZ, bZ = cx.psum[i3]
                        last = (B == 0)
                        kb.op("pe", lambda e: e.matmul(pZ[:], lhsT=kseg[ks][:, bl * 128:(bl + 1) * 128], rhs=qT[k2][:, qsl], start=True, stop=False),
                              reads=[b_kseg[ks], b_qT[k2]], writes=[bZ])
                        kb.op("act", lambda e: e.activation(te[i3][:], pZ[:], AF.Exp), reads=[bZ], writes=[b_te[i3]])
                        kb.op("act", lambda e: e.activation(te[i3][:], te[i3][:], AF.Ln, bias=1.0, scale=1.0), reads=[b_te[i3]], writes=[b_te[i3]])
                        kb.op("dve", lambda e: e.scalar_tensor_tensor(tsm[i3][:], qpos[:, Q, :], kpos[:, B:B + 1], te[i3][:], op0=ALU.is_gt, op1=ALU.mult),
                              reads=[b_pos, b_te[i3]], writes=[b_tsm[i3]])
                        kb.op("pool", lambda e: e.tensor_copy(tsb[i3][:], tsm[i3][:]), reads=[b_tsm[i3]], writes=[b_tsb[i3]])
                        kb.op("pe", lambda e: e.matmul(pZ[:], lhsT=negU[:], rhs=tsb[i3][:], start=False, stop=first),
                              reads=[b_negU, b_tsb[i3]], writes=[bZ])
                        if not first:
                            kb.op("pe", lambda e: e.matmul(pZ[:], lhsT=negones[:], rhs=spacc[:], start=False, stop=True),
                                  reads=[b_negones, b_spacc], writes=[bZ])
                        kb.op("act", lambda e: e.activation(te[i3][:], pZ[:], AF.Exp), reads=[bZ], writes=[b_te[i3]])
                        kb.op("dve", lambda e: e.scalar_tensor_tensor(tw[i3][:], qpos[:, Q, :], kpos[:, B:B + 1], te[i3][:], op0=ALU.is_gt, op1=ALU.mult),
                              reads=[b_pos, b_te[i3]], writes=[b_tw[i3]])
                        kb.op("pe", lambda e: e.matmul(pO[:], lhsT=vseg[ks][:, bl, :], rhs=tw[i3][:], start=first, stop=last),
                              reads=[b_vseg[ks], b_tw[i3]], writes=[bO])
                        if not last:
                            if first:
                                kb.op("pool", lambda e: e.tensor_copy(spacc[:], tsm[i3][:]), reads=[b_tsm[i3]], writes=[b_spacc])
                            else:
                                kb.op("pool", lambda e: e.tensor_tensor(spacc[:], spacc[:], tsm[i3][:], op=ALU.add), reads=[b_tsm[i3], b_spacc], writes=[b_spacc])
                        first = False
                kb.op("act", lambda e: e.copy(mo[k2][:, qsl], pO[:]), reads=[bO], writes=[b_mo[k2]])
            kb.dma("sp", mix_dram[h * 128:(h + 1) * 128, :], mo[k2][:], reads=[b_mo[k2]], writes=[mix_buf])
        kT, vm, b_kv = memkv
        tmp = {"ex": [tw[0], tw[1]], "b_ex": [b_tw[0], b_tw[1]], "rsum": te[0], "b_rsum": b_te[0]}
        for hm in range(4):
            k2 = hm % 2

            def epi_q2(i, bi, p, bp):
                kb.op("act", lambda e: e.copy(qT[k2][:, bi * 512:(bi + 1) * 512], p[:]), reads=[bp], writes=[b_qT[k2]])
            emit_proj(cx, wq, [SB_H * 128 + hm * 128], inT, b_inT, tblocks, epi_q2, wk)
            emit_mem_attn_head(cx, c, hm, qT[k2], b_qT[k2], kT, vm, b_kv, lambda tsl, k2=k2: mo[k2][:, tsl], b_mo[k2], tmp)
            kb.dma("sp", mix_dram[(SB_H + hm) * 128:(SB_H + hm + 1) * 128, :], mo[k2][:], reads=[b_mo[k2]], writes=[mix_buf])
        kb.barrier()


def emit_out_transpose(cx, c, h2T, b_h2, out_dram, b_out):
    kb = cx.kb
    with ExitStack() as st:
        src = [cx.sb(st, f"ot_s{k}", [128, 512]) for k in range(3)]; b_src = [Buf() for _ in range(3)]
        dst = [cx.sb(st, f"ot_d{k}", [128, 4, 512]) for k in range(2)]; b_dst = [Buf(), Buf()]
        cnt = 0
        for tb in range(T // 512):
            for cg in range(NCH // 4):
                kd = (tb * 4 + cg) % 2
                for cc in range(4):
                    ch = cg * 4 + cc
                    k = cnt % 3; cnt += 1
                    kb.dma("sp", src[k][:], h2T[ch * 128:(ch + 1) * 128, tb * 512:(tb + 1) * 512], reads=[b_h2], writes=[b_src[k]])
                    p, bp = cx.psum[cnt % 4]
                    for tt in range(4):
                        kb.op("pe", lambda e, tt=tt, p=p, k=k: e.transpose(p[:, tt * 128:(tt + 1) * 128], src[k][:, tt * 128:(tt + 1) * 128], c["ident"][:]),
                              reads=[b_src[k], c["b_ident"]], writes=[bp])
                    kb.op("dve" if cc % 2 == 0 else "act",
                          (lambda e, p=p, kd=kd, cc=cc: e.tensor_copy(dst[kd][:, :, cc * 128:(cc + 1) * 128], p[:].rearrange("p (a n) -> p a n", a=4))) if cc % 2 == 0 else
                          (lambda e, p=p, kd=kd, cc=cc: e.copy(dst[kd][:, :, cc * 128:(cc + 1) * 128], p[:].rearrange("p (a n) -> p a n", a=4))),
                          reads=[bp], writes=[b_dst[kd]])
                kb.dma("sp", out_dram[tb * 512:(tb + 1) * 512, cg * 512:(cg + 1) * 512].rearrange("(a p) n -> p a n", p=128), dst[kd][:],
                       reads=[b_dst[kd]], writes=[b_out])
        kb.barrier()


def build_C(arrays, upto="all", n_exp=NE, n_heads=SB_H):
    nc = bass.Bass("TRN2", target_bir_lowering=False)
    dr = declare_inputs(nc, arrays)
    out = nc.dram_tensor("out", [T, D], F32, kind="ExternalOutput").ap()
    mix_dram = nc.dram_tensor("mix_dram", [D, T], BF16, kind="ExternalOutput" if upto == "mixer" else "Internal").ap()
    h1T = nc.dram_tensor("h1T", [D, T], F32, kind="Internal").ap()
    h2T = nc.dram_tensor("h2T", [D, T], F32, kind="Internal").ap()
    with ExitStack() as st:
        kb = KB(nc, st); cx = Ctx(nc, kb, st)
        c = load_consts(cx, st, dr)
        b_hin = Buf(); b_mix = Buf(); b_h1 = Buf(); b_h2 = Buf(); b_out = Buf()
        with ExitStack() as sm:
            inT = cx.sb(sm, "l1inT", [128, NCH, T], BF16); b_inT = [Buf() for _ in range(NCH)]
            with ExitStack() as s2:
                emit_load_fm_bf16(cx, s2, dr["hT_in"], b_hin, inT, b_inT)
                kb.barrier()
            memkv = emit_mem_kv(cx, c, sm, dr, 1)
            emit_l1_mixer(cx, c, dr, inT, b_inT, mix_dram, b_mix, memkv, n_heads=n_heads)
            kb.barrier()
        if upto != "mixer":
            emit_wout_ln1(cx, c, dr, 1, dr["b_w_out"], mix_dram, b_mix, dr["hT_in"], b_hin, h1T, b_h1)
            emit_moe_ln2(cx, c, dr, 1, h1T, b_h1, h2T, b_h2, n_exp=n_exp)
            emit_out_transpose(cx, c, h2T, b_h2, out, b_out)
        kb.barrier()
    return nc


def attn_consts(cidx):
    qp = (cidx * T + np.arange(T, dtype=np.float32)).reshape(1, 4, 512)
    qpos = np.ascontiguousarray(np.broadcast_to(qp, (128, 4, 512))).astype(np.float32)
    kpos = (np.arange(NKB, dtype=np.float32)[None, :] * 128 + np.arange(128, dtype=np.float32)[:, None]).astype(np.float32)
    j = np.arange(128)
    negU = -(j[:, None] >= j[None, :]).astype(np.float32)
    return qpos, kpos, negU


def _kernel_unfused(**inputs):
    inputs = {k: np.asarray(v) for k, v in inputs.items()}
    x = inputs["x"][0]
    cores = list(range(NCORES))
    base = dict(lru_host_inputs(inputs)); base["ident"] = np.eye(128, dtype=np.float32)
    mapsA = []
    for cidx in cores:
        m = dict(base); m["x_ext"] = x_ext_for_core(x, cidx); mapsA.append(m)
    ncA = build_A(mapsA[0])
    resA = run_bass_kernel_spmd(ncA, mapsA, core_ids=cores)
    summ_all = np.stack([r["summ"] for r in resA.results])
    del ncA, resA
    host0 = common_host_inputs(inputs, wl=0)
    mapsB = []
    for cidx in cores:
        m = dict(base); m.update(host0)
        m["x_ext"] = mapsA[cidx]["x_ext"]; m["summ_all"] = summ_all
        sel = np.zeros((128, NCORES), np.float32); sel[:, cidx] = 1.0; m["core_sel"] = sel
        m["mem"] = inputs["mem"][0]; m["mem_w_kv"] = inputs["mem_w_kv"]; m["a_w_out"] = inputs["a_w_out"][0]
        m["w_kv_shared"] = inputs["w_kv_shared"]
        mapsB.append(m)
    ncB = build_B(mapsB[0], upto="all")
    resB = run_bass_kernel_spmd(ncB, mapsB, core_ids=cores)
    h2 = [r["h2T"] for r in resB.results]
    kT_all = np.concatenate([r["kT_out"] for r in resB.results], axis=1)
    v_all = np.concatenate([r["v_out"] for r in resB.results], axis=0)
    del ncB, resB, mapsB, mapsA
    host1 = common_host_inputs(inputs, wl=1)
    mapsC = []
    for cidx in cores:
        qp, kpos, negU = attn_consts(cidx)
        m = dict(host1)
        m.update({"ident": base["ident"], "hT_in": h2[cidx], "kT_all": kT_all, "v_all": v_all, "qpos": qp, "kpos": kpos, "negU": negU,
                  "b_w_q": inputs["b_w_q"][0], "b_w_out": inputs["b_w_out"][0], "mem": inputs["mem"][0], "mem_w_kv": inputs["mem_w_kv"]})
        mapsC.append(m)
    ncC = build_C(mapsC[0], upto="all")
    resC = run_bass_kernel_spmd(ncC, mapsC, core_ids=cores)
    out = np.concatenate([r["out"] for r in resC.results], axis=0)
    return out.reshape(1, S, D).astype(np.float32)


def build_fused(arrays, n_exp=NE, n_heads=SB_H):
    nc = bass.Bass("TRN2", target_bir_lowering=False)
    dr = declare_inputs(nc, arrays)
    out = nc.dram_tensor("out", [T, D], F32, kind="ExternalOutput").ap()
    mix_dram = nc.dram_tensor("mix_dram", [D, T], BF16, kind="Internal").ap()
    hT0 = nc.dram_tensor("hT0", [D, HALO + T], F32, kind="Internal").ap()
    h1T = nc.dram_tensor("h1T", [D, T], F32, kind="Internal").ap()
    h2T = nc.dram_tensor("h2T", [D, T], F32, kind="Internal").ap()
    h3T = nc.dram_tensor("h3T", [D, T], F32, kind="Internal").ap()
    h4T = nc.dram_tensor("h4T", [D, T], F32, kind="Internal").ap()
    summ = nc.dram_tensor("summ", [128, 24], F32, kind="Internal").ap()
    summ_g = nc.dram_tensor("summ_g", [NCORES * 128, 24], F32, kind="Internal").ap()
    kT_own = nc.dram_tensor("kT_own", [SB_H * 128, T], BF16, kind="Internal").ap()
    v_own = nc.dram_tensor("v_own", [T, SB_H * 128], BF16, kind="Internal").ap()
    kT_g = nc.dram_tensor("kT_g", [NCORES * SB_H * 128, T], BF16, kind="Internal").ap()
    v_g = nc.dram_tensor("v_g", [NCORES * T, SB_H * 128], BF16, kind="Internal").ap()
    with ExitStack() as st:
        kb = KB(nc, st); cx = Ctx(nc, kb, st)
        c = load_consts(cx, st, dr)
        b_hT0 = Buf(); b_mix = Buf(); b_h1 = Buf(); b_h2 = Buf(); b_h3 = Buf(); b_h4 = Buf(); b_out = Buf()
        b_summ = Buf(); b_summg = Buf(); b_kvown = Buf(); b_kvg = Buf()
        with ExitStack() as sm:
            xT = cx.sb(sm, "xT", [128, NCH, HALO + T], BF16); b_xT = [Buf() for _ in range(NCH)]
            with ExitStack() as s2:
                emit_load_transpose(cx, c, s2, dr["x_ext"], HALO + T, xT, b_xT, 0, hT_dram=hT0, hT_buf=b_hT0)
                kb.barrier()
            emit_l0_mixer(cx, c, dr, xT, b_xT, None, None, None, None, summ_dram=summ.rearrange("p (n two) -> p n two", two=2), summ_buf=b_summ)
            kb.collective("AllGather", [summ], [summ_g], reads=[b_summ], writes=[b_summg])
            hin, b_hin = emit_carry(cx, c, sm, dr, summ_src=summ_g, summ_buf=b_summg)
            memkv = emit_mem_kv(cx, c, sm, dr, 0)
            emit_l0_mixer(cx, c, dr, xT, b_xT, mix_dram, b_mix, hin, b_hin, memkv=memkv)
            kb.barrier()
        emit_wout_ln1(cx, c, dr, 0, dr["a_w_out"], mix_dram, b_mix, hT0[:, HALO:], b_hT0, h1T, b_h1)
        emit_moe_ln2(cx, c, dr, 0, h1T, b_h1, h2T, b_h2, n_exp=n_exp, wl=0)
        emit_kv_proj(cx, c, dr, h2T, b_h2, kT_own, v_own, b_kvown)
        kb.collective("AllGather", [kT_own], [kT_g], reads=[b_kvown], writes=[b_kvg])
        kb.collective("AllGather", [v_own], [v_g], reads=[b_kvown], writes=[b_kvg])
        with ExitStack() as sm:
            inT = cx.sb(sm, "l1inT", [128, NCH, T], BF16); b_inT = [Buf() for _ in range(NCH)]
            with ExitStack() as s2:
                emit_load_fm_bf16(cx, s2, h2T, b_h2, inT, b_inT)
                kb.barrier()
            memkv = emit_mem_kv(cx, c, sm, dr, 1)
            emit_l1_mixer(cx, c, dr, inT, b_inT, mix_dram, b_mix, memkv, n_heads=n_heads, kv_src=(kT_g, v_g), kv_buf=b_kvg)
            kb.barrier()
        emit_wout_ln1(cx, c, dr, 1, dr["b_w_out"], mix_dram, b_mix, h2T, b_h2, h3T, b_h3)
        emit_moe_ln2(cx, c, dr, 1, h3T, b_h3, h4T, b_h4, n_exp=n_exp, wl=1)
        emit_out_transpose(cx, c, h4T, b_h4, out, b_out)
        kb.barrier()
    return nc


def kernel_unfused(**inputs):
    return _kernel_unfused(**inputs)


def fused_maps(inputs):
    x = inputs["x"][0]
    base = dict(lru_host_inputs(inputs)); base["ident"] = np.eye(128, dtype=np.float32)
    host = common_host_inputs(inputs)
    maps = []
    for cidx in range(NCORES):
        qp, kpos, negU = attn_consts(cidx)
        m = dict(base); m.update(host)
        sel = np.zeros((128, NCORES), np.float32); sel[:, cidx] = 1.0
        m.update({"x_ext": x_ext_for_core(x, cidx), "core_sel": sel, "mem": inputs["mem"][0], "mem_w_kv": inputs["mem_w_kv"],
                  "a_w_out": inputs["a_w_out"][0], "w_kv_shared": inputs["w_kv_shared"], "qpos": qp, "kpos": kpos, "negU": negU,
                  "b_w_q": inputs["b_w_q"][0], "b_w_out": inputs["b_w_out"][0]})
        maps.append(m)
    return maps


def kernel(**inputs):
    inputs = {k: np.asarray(v) for k, v in inputs.items()}
    maps = fused_maps(inputs)
    nc = build_fused(maps[0])
    res = run_bass_kernel_spmd(nc, maps, core_ids=list(range(NCORES)))
    out = np.concatenate([r["out"] for r in res.results], axis=0)
    return out.reshape(1, S, D).astype(np.float32)
```

```python
import numpy as np
from contextlib import ExitStack
import concourse.bass as bass
import concourse.mybir as mybir
from concourse.bass_utils import run_bass_kernel_spmd

F32 = mybir.dt.float32
BF16 = mybir.dt.bfloat16
AF = mybir.ActivationFunctionType
ALU = mybir.AluOpType
AX = mybir.AxisListType

NCORES = 8
D = 2048
S = 16384
T = S // NCORES
NCH = D // 128
NE = 32
TOPK = 4
DN_ALPHA = 4.0 ** 0.25
LN_EPS = 1e-5
LRU_W = 1536
MEM_W = 512
NMEM = 256
EPOCH = 30000


class Buf:
    __slots__ = ("name", "w", "r")

    def __init__(self, name=""):
        self.name = name
        self.w = None
        self.r = []


class KB:
    def __init__(self, nc, stack, n_dma_sems=48):
        self.nc = nc
        self.stack = stack
        self.eng = {"pe": nc.tensor, "dve": nc.vector, "act": nc.scalar, "pool": nc.gpsimd, "sp": nc.sync}
        self.cnt = {e: 0 for e in self.eng}
        self.sems = {e: [] for e in self.eng}
        self.seen = {e: {} for e in self.eng}
        self.cc_sem = stack.enter_context(nc.semaphore("cc_sem"))
        self.cc_val = 0
        self.cc_scratch = stack.enter_context(nc.sbuf_tensor("cc_scratch", [128, 8], F32))
        self.dma_sems = [stack.enter_context(nc.semaphore(f"dq{i}")) for i in range(n_dma_sems)]
        self.dma_val = [0] * n_dma_sems
        self.dma_rr = 0
        self.n_inst = 0

    def _eng_sem(self, e, count):
        ep = (count - 1) // EPOCH
        while len(self.sems[e]) <= ep:
            self.sems[e].append(self.stack.enter_context(self.nc.semaphore(f"s_{e}_{len(self.sems[e])}")))
        return self.sems[e][ep], count - ep * EPOCH

    def _resolve(self, e, tag):
        kind, key, value = tag
        if kind == "eng":
            if key == e and e in ("pe", "sp"):
                return None
            sem, v = self._eng_sem(key, value)
            sid = ("eng", key, (value - 1) // EPOCH)
        else:
            sem, v = self.dma_sems[key], value
            sid = ("dma", key)
        if self.seen[e].get(sid, 0) >= v:
            return None
        self.seen[e][sid] = v
        return sem, v

    def _wait(self, e, tag):
        r = self._resolve(e, tag)
        if r is not None:
            self.eng[e].wait_ge(r[0], r[1])
            self.n_inst += 1

    def _deps(self, e, reads, writes, extra=()):
        pend = []
        for t in extra:
            r = self._resolve(e, t)
            if r is not None:
                pend.append(r)
        for b in reads:
            if b.w is not None:
                r = self._resolve(e, b.w)
                if r is not None:
                    pend.append(r)
        for b in writes:
            if b.w is not None:
                r = self._resolve(e, b.w)
                if r is not None:
                    pend.append(r)
            for t in b.r:
                r = self._resolve(e, t)
                if r is not None:
                    pend.append(r)
        best = {}
        for sem, v in pend:
            k = id(sem)
            if k not in best or best[k][1] < v:
                best[k] = (sem, v)
        pend = list(best.values())
        for sem, v in pend:
            self.eng[e].wait_ge(sem, v)
            self.n_inst += 1
        return None

    def _commit(self, tag, reads, writes):
        for b in reads:
            b.r.append(tag)
            if len(b.r) > 48:
                last = {}
                for t in b.r:
                    last[(t[0], t[1])] = t
                b.r = list(last.values())
        for b in writes:
            b.w = tag
            b.r = []

    def op(self, e, fn, reads=(), writes=()):
        last = self._deps(e, reads, writes)
        self.cnt[e] += 1
        sem, v = self._eng_sem(e, self.cnt[e])
        ins = fn(self.eng[e])
        if last is not None:
            ins._wait_ge(last[0], last[1])
        ins.then_inc(sem, 1)
        self.n_inst += 1
        self._commit(("eng", e, self.cnt[e]), reads, writes)
        return ins

    def dma(self, q, out, in_, reads=(), writes=(), **kw):
        k = self.dma_rr
        self.dma_rr = (self.dma_rr + 1) % len(self.dma_sems)
        extra = [("dma", k, self.dma_val[k])] if self.dma_val[k] > 0 else []
        last = self._deps(q, reads, writes, extra=extra)
        self.dma_val[k] += 16
        ins = self.eng[q].dma_start(out=out, in_=in_, **kw)
        if last is not None:
            ins._wait_ge(last[0], last[1])
        ins.then_inc(self.dma_sems[k], 16)
        self.n_inst += 1
        tag = ("dma", k, self.dma_val[k])
        self._commit(tag, reads, writes)
        return tag

    def collective(self, kind, ins, outs, reads=(), writes=(), groups=None):
        q = "pool"
        last = self._deps(q, reads, writes)
        if last is not None:
            self.eng[q].wait_ge(last[0], last[1])
        self.cc_val += 1
        ins_ = self.eng[q].collective_compute(kind, ALU.bypass, replica_groups=groups or [list(range(NCORES))],
                                              ins=[a.opt() for a in ins], outs=[a.opt() for a in outs])
        ins_.then_inc(self.cc_sem)
        self.eng[q].wait_ge(self.cc_sem, self.cc_val)
        self.n_inst += 2
        self.cnt[q] += 1
        sem, v = self._eng_sem(q, self.cnt[q])
        self.eng[q].memset(self.cc_scratch[:], 0.0).then_inc(sem, 1)
        tag = ("eng", q, self.cnt[q])
        self._commit(tag, reads, writes)
        return tag

    def barrier(self):
        for e in self.eng:
            for o in self.eng:
                if o != e and self.cnt[o] > 0:
                    self._wait(e, ("eng", o, self.cnt[o]))
            for k, v in enumerate(self.dma_val):
                if v > 0:
                    self._wait(e, ("dma", k, v))


class Ctx:
    def __init__(self, nc, kb, stack):
        self.nc, self.kb, self.stack = nc, kb, stack
        self.psum = []
        for i in range(8):
            t = stack.enter_context(nc.psum_tensor(f"ps{i}", [128, 512], F32))
            self.psum.append((t, Buf(f"ps{i}")))

    def sb(self, st, name, shape, dt=F32):
        self.uid = getattr(self, "uid", 0) + 1
        return st.enter_context(self.nc.sbuf_tensor(f"sb{self.uid}_{name}", shape, dt))


def load_consts(cx, st, dr):
    kb = cx.kb
    c = {}
    c["ident"] = cx.sb(st, "ident", [128, 128]); c["b_ident"] = Buf()
    kb.dma("sp", c["ident"][:], dr["ident"], writes=[c["b_ident"]])
    c["ones"] = cx.sb(st, "ones", [128, 128]); c["b_ones"] = Buf()
    kb.op("pool", lambda e: e.memset(c["ones"][:], 1.0), writes=[c["b_ones"]])
    c["ones_bf"] = cx.sb(st, "ones_bf", [128, 128], BF16); c["b_ones_bf"] = Buf()
    kb.op("pool", lambda e: e.memset(c["ones_bf"][:], 1.0), writes=[c["b_ones_bf"]])
    return c


def emit_ln(cx, c, ws, v_aps, v_bufs, g_ap, b_ap, gb_buf, out_dram, out_buf, tsl, psA, psB):
    kb = cx.kb
    sq, b_sq = ws["sq"], ws["b_sq"]
    (pA, bA), (pB, bB) = psA, psB
    for ch in range(NCH):
        k = ch % 2
        kb.op("act", lambda e, ch=ch, k=k: e.activation(sq[k][:], v_aps[ch], AF.Square), reads=[v_bufs[ch]], writes=[b_sq[k]])
        kb.op("pe", lambda e, ch=ch: e.matmul(pA[:], lhsT=c["ones"][:], rhs=v_aps[ch], start=(ch == 0), stop=(ch == NCH - 1)),
              reads=[c["b_ones"], v_bufs[ch]], writes=[bA])
        kb.op("pe", lambda e, ch=ch, k=k: e.matmul(pB[:], lhsT=c["ones"][:], rhs=sq[k][:], start=(ch == 0), stop=(ch == NCH - 1)),
              reads=[c["b_ones"], b_sq[k]], writes=[bB])
    mean, b_mean = ws["mean"], ws["b_mean"]
    rstd, b_rstd = ws["rstd"], ws["b_rstd"]
    kb.op("act", lambda e: e.mul(mean[:], pA[:], 1.0 / D), reads=[bA], writes=[b_mean])
    kb.op("dve", lambda e: e.tensor_tensor(rstd[:], mean[:], mean[:], op=ALU.mult), reads=[b_mean], writes=[b_rstd])
    kb.op("dve", lambda e: e.scalar_tensor_tensor(rstd[:], pB[:], 1.0 / D, rstd[:], op0=ALU.mult, op1=ALU.subtract), reads=[bB, b_rstd], writes=[b_rstd])
    kb.op("dve", lambda e: e.tensor_scalar(rstd[:], rstd[:], LN_EPS, None, op0=ALU.add), reads=[b_rstd], writes=[b_rstd])
    kb.op("act", lambda e: e.activation(rstd[:], rstd[:], AF.Sqrt), reads=[b_rstd], writes=[b_rstd])
    kb.op("dve", lambda e: e.reciprocal(rstd[:], rstd[:]), reads=[b_rstd], writes=[b_rstd])
    for ch in range(NCH):
        k = ch % 2
        t1, b_t1 = ws["t1"][k], ws["b_t1"][k]
        kb.op("dve", lambda e, ch=ch, t1=t1: e.tensor_tensor(t1[:], v_aps[ch], mean[:], op=ALU.subtract), reads=[v_bufs[ch], b_mean], writes=[b_t1])
        kb.op("pool", lambda e, t1=t1: e.tensor_tensor(t1[:], t1[:], rstd[:], op=ALU.mult), reads=[b_t1, b_rstd], writes=[b_t1])
        kb.op("act", lambda e, ch=ch, t1=t1: e.activation(t1[:], t1[:], AF.Identity, scale=g_ap[:, ch:ch + 1], bias=b_ap[:, ch:ch + 1]),
              reads=[b_t1, gb_buf], writes=[b_t1])
        kb.dma("sp", out_dram[ch * 128:(ch + 1) * 128, tsl], t1[:], reads=[b_t1], writes=[out_buf])


def alloc_ln_ws(cx, st, pfx):
    ws = {}
    ws["sq"] = [cx.sb(st, f"{pfx}sq{k}", [128, 512]) for k in range(2)]
    ws["b_sq"] = [Buf() for _ in range(2)]
    ws["mean"] = cx.sb(st, f"{pfx}mean", [128, 512]); ws["b_mean"] = Buf()
    ws["rstd"] = cx.sb(st, f"{pfx}rstd", [128, 512]); ws["b_rstd"] = Buf()
    ws["t1"] = [cx.sb(st, f"{pfx}t1{k}", [128, 512]) for k in range(2)]
    ws["b_t1"] = [Buf() for _ in range(2)]
    return ws


PASS = 1024


def emit_moe_ln2(cx, c, dr, lay, h1T_dram, h1_buf, h2T_dram, h2_buf, n_exp=NE, n_pass=T // PASS, wl=0):
    kb, nc = cx.kb, cx.nc
    with ExitStack() as st:
        sb = lambda name, shape, dt=F32: cx.sb(st, name, shape, dt)
        acc = sb("acc", [128, NCH, PASS]); b_acc = [[Buf() for _ in range(2)] for _ in range(NCH)]
        xT = sb("xT", [128, NCH, PASS], BF16); b_xT = [Buf() for _ in range(NCH)]
        actT = sb("actT", [128, NCH, PASS], BF16); b_actT = [[Buf() for _ in range(2)] for _ in range(NCH)]
        stg = [sb(f"stg{k}", [128, NCH // 2, 256]) for k in range(2)]; b_stg = [Buf() for _ in range(2)]
        wbf = [sb(f"wbf{k}", [128, NCH, 256], BF16) for k in range(2)]; b_wbf = [Buf() for _ in range(2)]
        G = sb("G", [128, PASS]); b_G = [Buf() for _ in range(2)]
        tg = [sb(f"tg{k}", [128, 512]) for k in range(2)]; b_tg = [Buf() for _ in range(2)]
        tsg = [sb(f"tsg{k}", [128, 512]) for k in range(2)]; b_tsg = [Buf() for _ in range(2)]
        tu = [sb(f"tu{k}", [128, 512]) for k in range(2)]; b_tu = [Buf() for _ in range(2)]
        wr = sb("wr", [128, NCH, NE]); b_wr = Buf()
        rb = sb("rb", [128, NE]); b_rb = Buf()
        bg = sb("bg", [128, NE * NCH]); bu1 = sb("bu1", [128, NE * NCH]); b_bgu = Buf()
        lng = sb("lng", [128, NCH]); lnb = sb("lnb", [128, NCH]); b_lngb = Buf()
        gatesT = sb("gatesT", [32, PASS]); b_gatesT = Buf()
        gsel = sb("gsel", [32, PASS]); b_gsel = Buf()
        rt = {n: sb("rt_" + n, [128, NE]) for n in ("l", "e", "m", "em", "g")}
        rs = {n: sb("rs_" + n, [128, 8]) for n in ("m8", "nm1", "ss", "rs")}
        b_rt = Buf()
        ws = {"sq": tg, "b_sq": b_tg, "t1": tu, "b_t1": b_tu, "mean": tsg[0], "b_mean": b_tsg[0], "rstd": tsg[1], "b_rstd": b_tsg[1]}

        kb.dma("sp", wr[:], dr["router_w"][lay].rearrange("(c p) e -> p c e", p=128), writes=[b_wr])
        kb.dma("sp", rb[:], dr["router_b_bc"][lay], writes=[b_rb])
        kb.dma("sp", bg[:], dr["bg_fm"][lay], writes=[b_bgu])
        kb.dma("sp", bu1[:], dr["bu_fm"][lay], writes=[b_bgu])
        kb.op("pool", lambda e: e.tensor_scalar(bu1[:], bu1[:], 1.0, None, op0=ALU.add), reads=[b_bgu], writes=[b_bgu])
        kb.dma("sp", lng[:], dr["ln2_g_fm"][lay], writes=[b_lngb])
        kb.dma("sp", lnb[:], dr["ln2_b_fm"][lay], writes=[b_lngb])

        wgu = dr["w_gate_up"][wl]
        wdn = dr["w_down"][wl]
        cast_rr = [0]

        for ps_i in range(n_pass):
            t0 = ps_i * PASS
            for ch in range(NCH):
                kb.dma("sp", acc[:, ch, :], h1T_dram[ch * 128:(ch + 1) * 128, t0:t0 + PASS], reads=[h1_buf], writes=b_acc[ch])
            for ch in range(NCH):
                eng = ("pool", "act", "dve")[ch % 3]
                if eng == "act":
                    kb.op("act", lambda e, ch=ch: e.copy(xT[:, ch, :], acc[:, ch, :]), reads=b_acc[ch], writes=[b_xT[ch]])
                else:
                    kb.op(eng, lambda e, ch=ch: e.tensor_copy(xT[:, ch, :], acc[:, ch, :]), reads=b_acc[ch], writes=[b_xT[ch]])
            pR, bR = cx.psum[6]
            pT, bT = cx.psum[7]
            for tt in range(PASS // 128):
                for ch in range(NCH):
                    kb.op("pe", lambda e, ch=ch, tt=tt: e.matmul(pR[:, 0:NE], lhsT=acc[:, ch, tt * 128:(tt + 1) * 128], rhs=wr[:, ch, :],
                                                                 start=(ch == 0), stop=(ch == NCH - 1)),
                          reads=[b_acc[ch][tt // 4], b_wr], writes=[bR])
                kb.op("dve", lambda e: e.tensor_tensor(rt["l"][:], pR[:, 0:NE], rb[:], op=ALU.add), reads=[bR, b_rb], writes=[b_rt])
                kb.op("dve", lambda e: e.max(rs["m8"][:], rt["l"][:]), reads=[b_rt], writes=[b_rt])
                kb.op("dve", lambda e: e.tensor_scalar(rs["nm1"][:, 0:1], rs["m8"][:, 0:1], -1.0, None, op0=ALU.mult), reads=[b_rt], writes=[b_rt])
                kb.op("act", lambda e: e.activation(rt["e"][:], rt["l"][:], AF.Exp, bias=rs["nm1"][:, 0:1], scale=1.0), reads=[b_rt], writes=[b_rt])
                kb.op("dve", lambda e: e.tensor_scalar(rt["m"][:], rt["l"][:], rs["m8"][:, 3:4], None, op0=ALU.is_ge), reads=[b_rt], writes=[b_rt])
                kb.op("dve", lambda e: e.tensor_tensor(rt["em"][:], rt["e"][:], rt["m"][:], op=ALU.mult), reads=[b_rt], writes=[b_rt])
                kb.op("dve", lambda e: e.reduce_sum(rs["ss"][:, 0:1], rt["em"][:], axis=AX.X), reads=[b_rt], writes=[b_rt])
                kb.op("dve", lambda e: e.reciprocal(rs["rs"][:, 0:1], rs["ss"][:, 0:1]), reads=[b_rt], writes=[b_rt])
                kb.op("dve", lambda e: e.tensor_scalar(rt["g"][:], rt["em"][:], rs["rs"][:, 0:1], None, op0=ALU.mult), reads=[b_rt], writes=[b_rt])
                kb.op("pe", lambda e: e.transpose(pT[0:NE, 0:128], rt["g"][:], c["ident"][:]), reads=[b_rt, c["b_ident"]], writes=[bT])
                kb.op("act", lambda e, tt=tt: e.copy(gatesT[:, tt * 128:(tt + 1) * 128], pT[0:NE, 0:128]), reads=[bT], writes=[b_gatesT])
            for ch in range(NCH):
                for hf in range(2):
                    kb.op("pool", lambda e, ch=ch, hf=hf: e.tensor_scalar(acc[:, ch, hf * 512:(hf + 1) * 512], acc[:, ch, hf * 512:(hf + 1) * 512],
                                                                        DN_ALPHA, None, op0=ALU.mult),
                          reads=[], writes=[b_acc[ch][hf]])
            bdT = stg[0][0:32, :, :].rearrange("p a b -> p (a b)")
            kb.dma("sp", bdT, dr["b_down"][lay], writes=[b_stg[0]])
            for db in range(NCH):
                for tb in range(2):
                    pB_, bB_ = cx.psum[4 + (db * 2 + tb) % 2]
                    kb.op("pe", lambda e, db=db, tb=tb, pB_=pB_: e.matmul(pB_[:], lhsT=bdT[:, db * 128:(db + 1) * 128], rhs=gatesT[:, tb * 512:(tb + 1) * 512],
                                                                         start=True, stop=True),
                          reads=[b_stg[0], b_gatesT], writes=[bB_])
                    kb.op("dve", lambda e, db=db, tb=tb, pB_=pB_: e.tensor_tensor(acc[:, db, tb * 512:(tb + 1) * 512], acc[:, db, tb * 512:(tb + 1) * 512], pB_[:], op=ALU.add),
                          reads=[bB_], writes=[b_acc[db][tb]])

            pieces = []
            for ex in range(n_exp):
                for j in range(NCH):
                    pieces.append(("gu", ex, j))
                for dp in range(NCH // 2):
                    pieces.append(("dn", ex, dp))

            def issue_dma(i):
                kind, ex, j = pieces[i]
                k = i % 2
                src = wgu[ex] if kind == "gu" else wdn[ex]
                srcv = src[:, j * 256:(j + 1) * 256].rearrange("(c p) n -> p c n", p=128)
                h = NCH // 2
                kb.dma("sp", stg[0][:], srcv[:, 0:h, :], writes=[b_stg[0]])
                kb.dma("sp", stg[1][:], srcv[:, h:, :], writes=[b_stg[1]])

            def issue_cast(i):
                kind, ex, j = pieces[i]
                k = i % 2
                groups = [("pool", 0, 5), ("dve", 5, 8), ("dve", 8, 10), ("act", 10, 16)]
                for eng, a, b in groups:
                    hh = 0 if a < 8 else 1
                    a2, b2 = a - 8 * hh, b - 8 * hh
                    if kind == "gu":
                        src_ap = stg[hh][:, a2:b2, :].rearrange("p c (i two) -> p c two i", two=2)
                        dst_ap = wbf[k][:, a:b, :].rearrange("p c (two i) -> p c two i", two=2)
                    else:
                        src_ap = stg[hh][:, a2:b2, :]
                        dst_ap = wbf[k][:, a:b, :]
                    if eng == "act":
                        kb.op("act", lambda e, s=src_ap, d=dst_ap: e.copy(d, s), reads=[b_stg[hh]], writes=[b_wbf[k]])
                    else:
                        kb.op(eng, lambda e, s=src_ap, d=dst_ap: e.tensor_copy(d, s), reads=[b_stg[hh]], writes=[b_wbf[k]])

            def compute(i):
                kind, ex, j = pieces[i]
                k = i % 2
                if kind == "gu":
                    if j == 0:
                        kb.op("pool", lambda e: e.tensor_scalar(gsel[:], gatesT[:], c["ident"][0:32, ex:ex + 1], None, op0=ALU.mult),
                              reads=[b_gatesT, c["b_ident"]], writes=[b_gsel])
                        for tb in range(2):
                            pG, bG = cx.psum[6 + tb]
                            kb.op("pe", lambda e, tb=tb, pG=pG: e.matmul(pG[:], lhsT=c["ones"][0:32, :], rhs=gsel[:, tb * 512:(tb + 1) * 512], start=True, stop=True),
                                  reads=[c["b_ones"], b_gsel], writes=[bG])
                            kb.op("act", lambda e, tb=tb, pG=pG: e.copy(G[:, tb * 512:(tb + 1) * 512], pG[:]), reads=[bG], writes=[b_G[tb]])
                    for tb in range(2):
                        q = (j * 2 + tb) % 2
                        pg, bpg = cx.psum[q * 2]
                        pu, bpu = cx.psum[q * 2 + 1]
                        tsl = slice(tb * 512, (tb + 1) * 512)
                        for ch in range(NCH):
                            kb.op("pe", lambda e, ch=ch, pg=pg: e.matmul(pg[:], lhsT=wbf[k][:, ch, 0:128], rhs=xT[:, ch, tsl], start=(ch == 0), stop=(ch == NCH - 1)),
                                  reads=[b_wbf[k], b_xT[ch]], writes=[bpg])
                        for ch in range(NCH):
                            kb.op("pe", lambda e, ch=ch, pu=pu: e.matmul(pu[:], lhsT=wbf[k][:, ch, 128:256], rhs=xT[:, ch, tsl], start=(ch == 0), stop=(ch == NCH - 1)),
                                  reads=[b_wbf[k], b_xT[ch]], writes=[bpu])
                        col = ex * NCH + j
                        kb.op("dve", lambda e, pg=pg, q=q: e.tensor_scalar(tg[q][:], pg[:], bg[:, col:col + 1], 7.0, op0=ALU.add, op1=ALU.min),
                              reads=[bpg, b_bgu], writes=[b_tg[q]])
                        kb.op("act", lambda e, q=q: e.activation(tsg[q][:], tg[q][:], AF.Sigmoid, scale=1.702), reads=[b_tg[q]], writes=[b_tsg[q]])
                        kb.op("act", lambda e, pu=pu, q=q: e.activation(tu[q][:], pu[:], AF.Identity, bias=bu1[:, col:col + 1], scale=1.0),
                              reads=[bpu, b_bgu], writes=[b_tu[q]])
                        kb.op("pool", lambda e, q=q: e.tensor_scalar(tu[q][:], tu[q][:], 8.0, -6.0, op0=ALU.min, op1=ALU.max), reads=[b_tu[q]], writes=[b_tu[q]])
                        kb.op("pool", lambda e, q=q: e.tensor_tensor(tsg[q][:], tsg[q][:], tg[q][:], op=ALU.mult), reads=[b_tsg[q], b_tg[q]], writes=[b_tsg[q]])
                        kb.op("pool", lambda e, q=q: e.tensor_tensor(tu[q][:], tu[q][:], tsg[q][:], op=ALU.mult), reads=[b_tu[q], b_tsg[q]], writes=[b_tu[q]])
                        kb.op("dve", lambda e, q=q, tsl=tsl: e.tensor_tensor(actT[:, j, tsl], tu[q][:], G[:, tsl], op=ALU.mult),
                              reads=[b_tu[q], b_G[tb]], writes=[b_actT[j][tb]])
                else:
                    dp = j
                    for dbl in range(2):
                        db = dp * 2 + dbl
                        for tb in range(2):
                            pd, bpd = cx.psum[4 + (dbl * 2 + tb) % 2]
                            tsl = slice(tb * 512, (tb + 1) * 512)
                            for ch in range(NCH):
                                kb.op("pe", lambda e, ch=ch, pd=pd: e.matmul(pd[:], lhsT=wbf[k][:, ch, dbl * 128:(dbl + 1) * 128], rhs=actT[:, ch, tsl],
                                                                             start=(ch == 0), stop=(ch == NCH - 1)),
                                      reads=[b_wbf[k], b_actT[ch][tb]], writes=[bpd])
                            kb.op("dve", lambda e, pd=pd, db=db, tsl=tsl: e.tensor_tensor(acc[:, db, tsl], acc[:, db, tsl], pd[:], op=ALU.add),
                                  reads=[bpd], writes=[b_acc[db][tb]])

            n = len(pieces)
            if n > 0:
                issue_dma(0)
                issue_cast(0)
                if n > 1:
                    issue_dma(1)
                for i in range(n):
                    if i + 1 < n:
                        issue_cast(i + 1)
                    if i + 2 < n:
                        issue_dma(i + 2)
                    compute(i)

            for tb in range(2):
                tsl = slice(tb * 512, (tb + 1) * 512)
                v_aps = [acc[:, ch, tsl] for ch in range(NCH)]
                v_bufs = [b_acc[ch][tb] for ch in range(NCH)]
                emit_ln(cx, c, ws, v_aps, v_bufs, lng, lnb, b_lngb, h2T_dram, h2_buf, slice(t0 + tb * 512, t0 + (tb + 1) * 512), cx.psum[6], cx.psum[7])
        kb.barrier()


def fm(v):
    sh = v.shape
    return np.ascontiguousarray(v.reshape(sh[:-1] + (sh[-1] // 128, 128)).swapaxes(-1, -2))


def common_host_inputs(inputs, wl=None):
    h = {}
    h["ident"] = np.eye(128, dtype=np.float32)
    h["router_w"] = np.ascontiguousarray(inputs["router_w"])
    h["router_b_bc"] = np.ascontiguousarray(np.broadcast_to(inputs["router_b"][:, None, :], (2, 128, NE)))
    bgu = inputs["b_gate_up"]
    bgate = bgu[:, :, 0::2]
    bup = bgu[:, :, 1::2]
    h["bg_fm"] = np.ascontiguousarray(bgate.reshape(2, NE, NCH, 128).transpose(0, 3, 1, 2).reshape(2, 128, NE * NCH))
    h["bu_fm"] = np.ascontiguousarray(bup.reshape(2, NE, NCH, 128).transpose(0, 3, 1, 2).reshape(2, 128, NE * NCH))
    h["ln1_g_fm"] = fm(inputs["ln1_g"]); h["ln1_b_fm"] = fm(inputs["ln1_b"])
    h["ln2_g_fm"] = fm(inputs["ln2_g"]); h["ln2_b_fm"] = fm(inputs["ln2_b"])
    h["b_down"] = np.ascontiguousarray(inputs["b_down"])
    if wl is None:
        h["w_gate_up"] = inputs["w_gate_up"]
        h["w_down"] = inputs["w_down"]
    else:
        h["w_gate_up"] = inputs["w_gate_up"][wl:wl + 1]
        h["w_down"] = inputs["w_down"][wl:wl + 1]
    return h


def declare_inputs(nc, arrays):
    dr = {}
    for k, v in arrays.items():
        dt = {np.dtype(np.float32): F32}.get(v.dtype, None)
        if dt is None:
            dt = BF16
        dr[k] = nc.dram_tensor(k, list(v.shape), dt, kind="ExternalInput").ap()
    return dr


def emit_proj(cx, w_dram, cols, inT, b_inT, tblocks, epilogue, wk, ps_banks=(0, 1, 2, 3), post_block=None):
    kb = cx.kb
    stg, b_stg, wbf, b_wbf = wk
    n = len(cols)

    def issue_dma(i):
        k = i % 2
        srcv = w_dram[:, cols[i]:cols[i] + 128].rearrange("(c p) n -> p c n", p=128)
        kb.dma("sp", stg[k][:], srcv, writes=[b_stg[k]])

    def issue_cast(i):
        k = i % 2
        kb.op("pool", lambda e: e.tensor_copy(wbf[k][:, 0:6, :], stg[k][:, 0:6, :]), reads=[b_stg[k]], writes=[b_wbf[k]])
        kb.op("act", lambda e: e.copy(wbf[k][:, 6:11, :], stg[k][:, 6:11, :]), reads=[b_stg[k]], writes=[b_wbf[k]])
        kb.op("dve", lambda e: e.tensor_copy(wbf[k][:, 11:16, :], stg[k][:, 11:16, :]), reads=[b_stg[k]], writes=[b_wbf[k]])

    issue_dma(0)
    issue_cast(0)
    if n > 1:
        issue_dma(1)
    cnt = 0
    for i in range(n):
        k = i % 2
        if i + 1 < n:
            issue_cast(i + 1)
        if i + 2 < n:
            issue_dma(i + 2)
        for bi, (t0, tn) in enumerate(tblocks):
            p, bp = cx.psum[ps_banks[cnt % len(ps_banks)]]
            cnt += 1
            for ch in range(NCH):
                kb.op("pe", lambda e, ch=ch: e.matmul(p[:, 0:tn], lhsT=wbf[k][:, ch, :], rhs=inT[:, ch, t0:t0 + tn], start=(ch == 0), stop=(ch == NCH - 1)),
                      reads=[b_wbf[k], b_inT[ch]], writes=[bp])
            epilogue(i, bi, p, bp)
        if post_block is not None:
            post_block(i)


def alloc_wk(cx, st, pfx):
    stg = [cx.sb(st, f"{pfx}stg{k}", [128, NCH, 128]) for k in range(2)]
    wbf = [cx.sb(st, f"{pfx}wbf{k}", [128, NCH, 128], BF16) for k in range(2)]
    return stg, [Buf(), Buf()], wbf, [Buf(), Buf()]


def emit_mem_attn_head(cx, c, h, qT, b_qT, kT, vm, b_kv, out_ap_fn, b_out, tmp, ntok=T):
    kb = cx.kb
    ex, b_ex = tmp["ex"], tmp["b_ex"]
    rsum, b_rsum = tmp["rsum"], tmp["b_rsum"]
    for tb in range(ntok // 512):
        tsl = slice(tb * 512, (tb + 1) * 512)
        for mb in range(2):
            pS, bS = cx.psum[4 + mb]
            kb.op("pe", lambda e, mb=mb, pS=pS: e.matmul(pS[:], lhsT=kT[:, h, mb * 128:(mb + 1) * 128], rhs=qT[:, tsl], start=True, stop=True),
                  reads=[b_kv, b_qT], writes=[bS])
            kb.op("act", lambda e, mb=mb, pS=pS: e.activation(ex[mb][:], pS[:], AF.Exp, scale=128.0 ** -0.5), reads=[bS], writes=[b_ex[mb]])
        pO, bO = cx.psum[6]
        pZ, bZ = cx.psum[7]
        for mb in range(2):
            kb.op("pe", lambda e, mb=mb: e.matmul(pO[:], lhsT=vm[:, mb, h * 128:(h + 1) * 128], rhs=ex[mb][:], start=(mb == 0), stop=(mb == 1)),
                  reads=[b_kv, b_ex[mb]], writes=[bO])
        for mb in range(2):
            kb.op("pe", lambda e, mb=mb: e.matmul(pZ[:], lhsT=c["ones_bf"][:], rhs=ex[mb][:], start=(mb == 0), stop=(mb == 1)),
                  reads=[c["b_ones_bf"], b_ex[mb]], writes=[bZ])
        kb.op("dve", lambda e: e.reciprocal(rsum[:], pZ[:]), reads=[bZ], writes=[b_rsum])
        kb.op("dve", lambda e: e.tensor_tensor(out_ap_fn(tsl), pO[:], rsum[:], op=ALU.mult), reads=[bO, b_rsum], writes=[b_out])


def emit_mem_kv(cx, c, st, dr, lay):
    kb = cx.kb
    kT = cx.sb(st, "memkT", [128, 4, NMEM], BF16)
    vm = cx.sb(st, "memv", [128, 2, MEM_W], BF16)
    b_kv = Buf()
    with ExitStack() as s2:
        memT = cx.sb(s2, "memT", [128, NCH, NMEM], BF16); b_memT = [Buf() for _ in range(NCH)]
        mtok = cx.sb(s2, "mtok", [128, D]); b_mtok = Buf()
        wk = alloc_wk(cx, s2, "mk")
        for mb in range(2):
            kb.dma("sp", mtok[:], dr["mem"][mb * 128:(mb + 1) * 128, :], writes=[b_mtok])
            for ch in range(NCH):
                p, bp = cx.psum[ch % 2]
                kb.op("pe", lambda e, ch=ch, p=p: e.transpose(p[:, 0:128], mtok[:, ch * 128:(ch + 1) * 128], c["ident"][:]), reads=[b_mtok, c["b_ident"]], writes=[bp])
                kb.op("dve", lambda e, ch=ch, p=p, mb=mb: e.tensor_copy(memT[:, ch, mb * 128:(mb + 1) * 128], p[:, 0:128]), reads=[bp], writes=[b_memT[ch]])
        wkv = dr["mem_w_kv"][lay]
        def epi_k(i, bi, p, bp):
            kb.op("act", lambda e: e.copy(kT[:, i, :], p[:, 0:NMEM]), reads=[bp], writes=[b_kv])
        emit_proj(cx, wkv, [hh * 128 for hh in range(4)], memT, b_memT, [(0, NMEM)], epi_k, wk)
        stg, b_stg, wbf, b_wbf = wk
        for nb in range(4):
            k = nb % 2
            kb.dma("sp", stg[k][:], wkv[:, MEM_W + nb * 128:MEM_W + (nb + 1) * 128].rearrange("(c p) n -> p c n", p=128), writes=[b_stg[k]])
            kb.op("dve", lambda e, k=k: e.tensor_copy(wbf[k][:], stg[k][:]), reads=[b_stg[k]], writes=[b_wbf[k]])
            for mb in range(2):
                p, bp = cx.psum[2 + mb]
                for ch in range(NCH):
                    kb.op("pe", lambda e, ch=ch, p=p, mb=mb, k=k: e.matmul(p[:, 0:128], lhsT=memT[:, ch, mb * 128:(mb + 1) * 128], rhs=wbf[k][:, ch, :],
                                                                           start=(ch == 0), stop=(ch == NCH - 1)),
                          reads=[b_memT[ch], b_wbf[k]], writes=[bp])
                kb.op("act", lambda e, p=p, mb=mb, nb=nb: e.copy(vm[:, mb, nb * 128:(nb + 1) * 128], p[:, 0:128]), reads=[bp], writes=[b_kv])
        kb.barrier()
    return kT, vm, b_kv


def emit_load_transpose(cx, c, st_tmp, src_rows, nrows, inT, b_inT, col0, hT_dram=None, hT_buf=None, hT_col0=0):
    kb = cx.kb
    rows = [cx.sb(st_tmp, f"ltrow{k}", [128, D]) for k in range(2)]; b_rows = [Buf(), Buf()]
    f32t = [cx.sb(st_tmp, f"ltf{k}", [128, 128]) for k in range(2)]; b_f = [Buf(), Buf()]
    nt = (nrows + 127) // 128
    cnt = 0
    for ti in range(nt):
        r0 = ti * 128
        rn = min(128, nrows - r0)
        k = ti % 2
        kb.dma("sp", rows[k][0:rn, :], src_rows[r0:r0 + rn, :], writes=[b_rows[k]])
        for ch in range(NCH):
            p, bp = cx.psum[cnt % 4]
            kb.op("pe", lambda e, ch=ch, p=p: e.transpose(p[:, 0:rn], rows[k][0:rn, ch * 128:(ch + 1) * 128], c["ident"][0:rn, 0:rn]),
                  reads=[b_rows[k], c["b_ident"]], writes=[bp])
            kb.op("act", lambda e, ch=ch, p=p: e.copy(inT[:, ch, col0 + r0:col0 + r0 + rn], p[:, 0:rn]), reads=[bp], writes=[b_inT[ch]])
            if hT_dram is not None:
                q = cnt % 2
                kb.op("dve", lambda e, p=p, q=q: e.tensor_copy(f32t[q][:, 0:rn], p[:, 0:rn]), reads=[bp], writes=[b_f[q]])
                kb.dma("sp", hT_dram[ch * 128:(ch + 1) * 128, hT_col0 + r0:hT_col0 + r0 + rn], f32t[q][:, 0:rn], reads=[b_f[q]], writes=[hT_buf])
            cnt += 1


def emit_load_fm_bf16(cx, st_tmp, hT_dram, hT_buf, inT, b_inT, ntok=T):
    kb = cx.kb
    tmpf = [cx.sb(st_tmp, f"lfm{k}", [128, ntok]) for k in range(2)]; b_t = [Buf(), Buf()]
    for ch in range(NCH):
        k = ch % 2
        kb.dma("sp", tmpf[k][:], hT_dram[ch * 128:(ch + 1) * 128, 0:ntok], reads=[hT_buf], writes=[b_t[k]])
        eng = ("pool", "dve")[ch % 2]
        kb.op(eng, lambda e, ch=ch, k=k: e.tensor_copy(inT[:, ch, 0:ntok], tmpf[k][:]), reads=[b_t[k]], writes=[b_inT[ch]])


def emit_wout_ln1(cx, c, dr, lay, w_out_dram, mix_dram, mix_buf, hT_in, hin_buf, h1T, h1_buf):
    kb = cx.kb
    with ExitStack() as s2:
        mixT = cx.sb(s2, "mixT", [128, NCH, T], BF16); b_mixT = [Buf() for _ in range(NCH)]
        for ch in range(NCH):
            kb.dma("sp", mixT[:, ch, :], mix_dram[ch * 128:(ch + 1) * 128, :], reads=[mix_buf], writes=[b_mixT[ch]])
        wk = alloc_wk(cx, s2, "wo")
        v = cx.sb(s2, "v_ln1", [128, NCH, 512]); b_v = [Buf() for _ in range(NCH)]
        res = [cx.sb(s2, f"res{k}", [128, 512]) for k in range(2)]; b_res = [Buf(), Buf()]
        lng = cx.sb(s2, "ln1g", [128, NCH]); lnb = cx.sb(s2, "ln1b", [128, NCH]); b_gb = Buf()
        kb.dma("sp", lng[:], dr["ln1_g_fm"][lay], writes=[b_gb])
        kb.dma("sp", lnb[:], dr["ln1_b_fm"][lay], writes=[b_gb])
        ws = alloc_ln_ws(cx, s2, "ln1")
        for tb in range(T // 512):
            tsl = slice(tb * 512, (tb + 1) * 512)

            def epi(i, bi, p, bp):
                k = i % 2
                kb.dma("sp", res[k][:], hT_in[i * 128:(i + 1) * 128, tsl], reads=[hin_buf], writes=[b_res[k]])
                kb.op("dve", lambda e: e.scalar_tensor_tensor(v[:, i, :], res[k][:], DN_ALPHA, p[:], op0=ALU.mult, op1=ALU.add),
                      reads=[b_res[k], bp], writes=[b_v[i]])
            emit_proj(cx, w_out_dram, [i * 128 for i in range(NCH)], mixT, b_mixT, [(tb * 512, 512)], epi, wk)
            emit_ln(cx, c, ws, [v[:, ch, :] for ch in range(NCH)], b_v, lng, lnb, b_gb, h1T, h1_buf, tsl, cx.psum[6], cx.psum[7])
        kb.barrier()


def emit_lru_params(cx, c, st, dr):
    kb = cx.kb
    P = {}
    b = Buf()
    for name in ("cw0", "cw1", "cw2", "cw3", "cb", "rgb", "igb", "lam"):
        P[name] = cx.sb(st, "lp_" + name, [128, 12])
        kb.dma("sp", P[name][:], dr["lru_" + name], writes=[b])
    t = {n: cx.sb(st, "lp_t" + n, [128, 12]) for n in ("a", "y", "z", "z2", "s")}
    lam = P["lam"]
    kb.op("dve", lambda e: e.tensor_scalar(t["a"][:], lam[:], -1.0, None, op0=ALU.mult), reads=[b], writes=[b])
    kb.op("dve", lambda e: e.tensor_tensor(t["y"][:], lam[:], t["a"][:], op=ALU.max), reads=[b], writes=[b])
    kb.op("act", lambda e: e.activation(t["y"][:], t["y"][:], AF.Exp, scale=-1.0), reads=[b], writes=[b])
    kb.op("dve", lambda e: e.tensor_scalar(t["z"][:], t["y"][:], 2.0, None, op0=ALU.add), reads=[b], writes=[b])
    kb.op("dve", lambda e: e.reciprocal(t["z"][:], t["z"][:]), reads=[b], writes=[b])
    kb.op("dve", lambda e: e.tensor_tensor(t["z"][:], t["z"][:], t["y"][:], op=ALU.mult), reads=[b], writes=[b])
    kb.op("dve", lambda e: e.tensor_tensor(t["z2"][:], t["z"][:], t["z"][:], op=ALU.mult), reads=[b], writes=[b])
    kb.op("dve", lambda e: e.tensor_scalar(t["s"][:], t["z2"][:], 1.0 / 17, 1.0 / 15, op0=ALU.mult, op1=ALU.add), reads=[b], writes=[b])
    for kk in (13, 11, 9, 7, 5, 3, 1):
        kb.op("dve", lambda e: e.tensor_tensor(t["s"][:], t["s"][:], t["z2"][:], op=ALU.mult), reads=[b], writes=[b])
        kb.op("dve", lambda e, kk=kk: e.tensor_scalar(t["s"][:], t["s"][:], 1.0 / kk, None, op0=ALU.add), reads=[b], writes=[b])
    kb.op("dve", lambda e: e.tensor_tensor(t["s"][:], t["s"][:], t["z"][:], op=ALU.mult), reads=[b], writes=[b])
    kb.op("dve", lambda e: e.tensor_scalar(t["s"][:], t["s"][:], 2.0, None, op0=ALU.mult), reads=[b], writes=[b])
    kb.op("dve", lambda e: e.tensor_scalar(t["a"][:], t["a"][:], 0.0, None, op0=ALU.max), reads=[b], writes=[b])
    kb.op("dve", lambda e: e.tensor_tensor(t["s"][:], t["s"][:], t["a"][:], op=ALU.add), reads=[b], writes=[b])
    P["nsc"] = cx.sb(st, "lp_nsc", [128, 12])
    kb.op("dve", lambda e: e.tensor_scalar(P["nsc"][:], t["s"][:], -8.0, None, op0=ALU.mult), reads=[b], writes=[b])
    P["buf"] = b
    return P


HALO = 8


def emit_l0_mixer(cx, c, dr, xT, b_xT, mix_dram, mix_buf, hin_ap, hin_buf, summ_dram=None, summ_buf=None, memkv=None):
    kb = cx.kb
    summary = summ_dram is not None
    w_in = dr["a_w_in"]
    TT = HALO + T
    with ExitStack() as st:
        P = emit_lru_params(cx, c, st, dr)
        pb = P["buf"]
        wk = alloc_wk(cx, st, "l0")
        rgw = [cx.sb(st, f"rgw{k}", [128, 128]) for k in range(2)]
        igw = [cx.sb(st, f"igw{k}", [128, 128]) for k in range(2)]
        b_gw = [Buf(), Buf()]
        u = cx.sb(st, "lru_u", [128, TT]); b_u = Buf()
        xc = cx.sb(st, "lru_xc", [128, T]); b_xc = Buf()
        ra = cx.sb(st, "lru_ra", [128, T]); b_ra = Buf()
        ii = cx.sb(st, "lru_i", [128, T]); b_ii = Buf()
        bb = cx.sb(st, "lru_b", [128, T]); b_bb = Buf()
        gl = cx.sb(st, "lru_gl", [128, T]); b_gl = Buf()
        g2 = cx.sb(st, "lru_g2", [128, T]); b_g2 = Buf()
        mo = [cx.sb(st, f"lru_mo{k}", [128, T], BF16) for k in range(2)]; b_mo = [Buf(), Buf()]
        sm = cx.sb(st, "lru_sm", [128, 12, 2]); b_sm = Buf()
        racc = cx.sb(st, "lru_racc", [128, 4]); b_racc = Buf()
        if summary:
            kb.op("pool", lambda e: e.memset(sm[:], 0.0), writes=[b_sm])
        tblocks = [(0, HALO)] + [(HALO + i * 512, 512) for i in range(T // 512)]

        for n in range(12):
            k = n % 2
            kb.dma("sp", rgw[k][:], dr["a_rg_w"][n], writes=[b_gw[k]])
            kb.dma("sp", igw[k][:], dr["a_ig_w"][n], writes=[b_gw[k]])
            cols = [LRU_W + n * 128] if summary else [LRU_W + n * 128, n * 128]

            def epi(i, bi, p, bp):
                t0, tn = tblocks[bi]
                if i == 0:
                    kb.op("act", lambda e: e.copy(u[:, t0:t0 + tn], p[:, 0:tn]), reads=[bp], writes=[b_u])
                else:
                    if bi == 0:
                        return
                    o0 = t0 - HALO
                    kb.op("act", lambda e: e.copy(gl[:, o0:o0 + tn], p[:, 0:tn]), reads=[bp], writes=[b_gl])
            emit_proj(cx, w_in, cols, xT, b_xT, tblocks, epi, wk)

            kb.op("dve", lambda e: e.tensor_scalar(xc[:], u[:, HALO:HALO + T], P["cw3"][:, n:n + 1], P["cb"][:, n:n + 1], op0=ALU.mult, op1=ALU.add),
                  reads=[b_u, pb], writes=[b_xc])
            for tap, nm in ((1, "cw2"), (2, "cw1"), (3, "cw0")):
                kb.op("dve", lambda e, tap=tap, nm=nm: e.scalar_tensor_tensor(xc[:], u[:, HALO - tap:HALO - tap + T], P[nm][:, n:n + 1], xc[:], op0=ALU.mult, op1=ALU.add),
                      reads=[b_u, pb, b_xc], writes=[b_xc])
            for tb in range(T // 512):
                tsl = slice(tb * 512, (tb + 1) * 512)
                pr, bpr = cx.psum[4 + tb % 2]
                pi, bpi = cx.psum[6 + tb % 2]
                kb.op("pe", lambda e, pr=pr: e.matmul(pr[:], lhsT=rgw[k][:], rhs=xc[:, tsl], start=True, stop=True), reads=[b_gw[k], b_xc], writes=[bpr])
                kb.op("pe", lambda e, pi=pi: e.matmul(pi[:], lhsT=igw[k][:], rhs=xc[:, tsl], start=True, stop=True), reads=[b_gw[k], b_xc], writes=[bpi])
                if summary:
                    kb.op("act", lambda e, pr=pr, tb=tb: e.activation(ra[:, tsl], pr[:], AF.Sigmoid, bias=P["rgb"][:, n:n + 1], scale=1.0, accum_out=racc[:, tb:tb + 1]),
                          reads=[bpr, pb], writes=[b_ra, b_racc])
                else:
                    kb.op("act", lambda e, pr=pr: e.activation(ra[:, tsl], pr[:], AF.Sigmoid, bias=P["rgb"][:, n:n + 1], scale=1.0), reads=[bpr, pb], writes=[b_ra])
                kb.op("act", lambda e, pi=pi: e.activation(ii[:, tsl], pi[:], AF.Sigmoid, bias=P["igb"][:, n:n + 1], scale=1.0), reads=[bpi, pb], writes=[b_ii])
            kb.op("act", lambda e: e.activation(ra[:], ra[:], AF.Exp, scale=P["nsc"][:, n:n + 1]), reads=[b_ra, pb], writes=[b_ra])
            kb.op("pool", lambda e: e.tensor_tensor(bb[:], ra[:], ra[:], op=ALU.mult), reads=[b_ra], writes=[b_bb])
            kb.op("pool", lambda e: e.tensor_scalar(bb[:], bb[:], -1.0, 1.0, op0=ALU.mult, op1=ALU.add), reads=[b_bb], writes=[b_bb])
            kb.op("act", lambda e: e.activation(bb[:], bb[:], AF.Sqrt), reads=[b_bb], writes=[b_bb])
            kb.op("pool", lambda e: e.tensor_tensor(ii[:], ii[:], xc[:], op=ALU.mult), reads=[b_ii, b_xc], writes=[b_ii])
            kb.op("pool", lambda e: e.tensor_tensor(bb[:], bb[:], ii[:], op=ALU.mult), reads=[b_bb, b_ii], writes=[b_bb])
            if summary:
                kb.op("dve", lambda e: e.tensor_tensor_scan(xc[:], ra[:], bb[:], 0.0, op0=ALU.mult, op1=ALU.add), reads=[b_ra, b_bb], writes=[b_xc])
                kb.op("dve", lambda e: e.tensor_copy(sm[:, n, 0:1], xc[:, T - 1:T]), reads=[b_xc], writes=[b_sm])
                kb.op("dve", lambda e: e.reduce_sum(sm[:, n, 1:2], racc[:, 0:4], axis=AX.X), reads=[b_racc], writes=[b_sm])
                continue
            kb.op("dve", lambda e: e.tensor_tensor_scan(xc[:], ra[:], bb[:], hin_ap[:, n:n + 1], op0=ALU.mult, op1=ALU.add), reads=[b_ra, b_bb, hin_buf], writes=[b_xc])
            kb.op("pool", lambda e: e.tensor_tensor(g2[:], gl[:], gl[:], op=ALU.mult), reads=[b_gl], writes=[b_g2])
            kb.op("pool", lambda e: e.tensor_scalar(g2[:], g2[:], 0.044715, 1.0, op0=ALU.mult, op1=ALU.add), reads=[b_g2], writes=[b_g2])
            kb.op("pool", lambda e: e.tensor_tensor(g2[:], g2[:], gl[:], op=ALU.mult), reads=[b_g2, b_gl], writes=[b_g2])
            kb.op("act", lambda e: e.activation(g2[:], g2[:], AF.Sigmoid, scale=1.5957691216057308), reads=[b_g2], writes=[b_g2])
            kb.op("pool", lambda e: e.tensor_tensor(g2[:], g2[:], gl[:], op=ALU.mult), reads=[b_g2, b_gl], writes=[b_g2])
            kb.op("dve", lambda e: e.tensor_tensor(mo[k][:], xc[:], g2[:], op=ALU.mult), reads=[b_xc, b_g2], writes=[b_mo[k]])
            kb.dma("sp", mix_dram[n * 128:(n + 1) * 128, :], mo[k][:], reads=[b_mo[k]], writes=[mix_buf])
        if summary:
            kb.dma("sp", summ_dram, sm[:], reads=[b_sm], writes=[summ_buf])
            kb.barrier()
            return
        kT, vm, b_kv = memkv
        qT = [cx.sb(st, f"qmT{k}", [128, T], BF16) for k in range(2)]; b_qT = [Buf(), Buf()]
        tmp = {"ex": [cx.sb(st, f"mex{k}", [128, 512], BF16) for k in range(2)], "b_ex": [Buf(), Buf()],
               "rsum": cx.sb(st, "mrs", [128, 512]), "b_rsum": Buf()}
        for h in range(4):
            k = h % 2

            def epi_q(i, bi, p, bp):
                t0, tn = tblocks[1 + bi]
                kb.op("act", lambda e: e.copy(qT[k][:, t0 - HALO:t0 - HALO + tn], p[:, 0:tn]), reads=[bp], writes=[b_qT[k]])
            emit_proj(cx, w_in, [2 * LRU_W + h * 128], xT, b_xT, tblocks[1:], epi_q, wk)
            emit_mem_attn_head(cx, c, h, qT[k], b_qT[k], kT, vm, b_kv, lambda tsl, k=k: mo[k][:, tsl], b_mo[k], tmp)
            kb.dma("sp", mix_dram[(12 + h) * 128:(13 + h) * 128, :], mo[k][:], reads=[b_mo[k]], writes=[mix_buf])
        kb.barrier()


def emit_carry(cx, c, st, dr, summ_src=None, summ_buf=None, lam_p=None):
    kb = cx.kb
    b = Buf()
    sa = cx.sb(st, "cy_sa", [128, NCORES, 12, 2])
    sel = cx.sb(st, "cy_sel", [128, NCORES])
    if lam_p is None:
        lam_p = emit_lru_params(cx, c, st, dr)
    pb = lam_p["buf"]
    if summ_src is None:
        kb.dma("sp", sa[:], dr["summ_all"].rearrange("k p n two -> p k n two"), writes=[b])
    else:
        kb.dma("sp", sa[:], summ_src.rearrange("(k p) (n two) -> p k n two", p=128, two=2), reads=[summ_buf], writes=[b])
    kb.dma("sp", sel[:], dr["core_sel"], writes=[b])
    H = cx.sb(st, "cy_H", [128, 12]); hin = cx.sb(st, "cy_hin", [128, 12]); Pk = cx.sb(st, "cy_P", [128, 12]); tmp = cx.sb(st, "cy_t", [128, 12])
    kb.op("dve", lambda e: e.memset(H[:], 0.0), writes=[b])
    kb.op("dve", lambda e: e.memset(hin[:], 0.0), reads=[b], writes=[b])
    for k in range(NCORES):
        kb.op("dve", lambda e, k=k: e.scalar_tensor_tensor(hin[:], H[:], sel[:, k:k + 1], hin[:], op0=ALU.mult, op1=ALU.add), reads=[b], writes=[b])
        if k == NCORES - 1:
            break
        kb.op("dve", lambda e, k=k: e.tensor_tensor(Pk[:], sa[:, k, :, 1], lam_p["nsc"][:], op=ALU.mult), reads=[b, pb], writes=[b])
        kb.op("act", lambda e: e.activation(Pk[:], Pk[:], AF.Exp), reads=[b], writes=[b])
        kb.op("dve", lambda e: e.tensor_tensor(tmp[:], Pk[:], H[:], op=ALU.mult), reads=[b], writes=[b])
        kb.op("dve", lambda e, k=k: e.tensor_tensor(H[:], tmp[:], sa[:, k, :, 0], op=ALU.add), reads=[b], writes=[b])
    return hin, b


def lru_host_inputs(inputs):
    h = {}
    cw = inputs["a_conv_w"][0]
    for i in range(4):
        h[f"lru_cw{i}"] = fm(cw[i])
    h["lru_cb"] = fm(inputs["a_conv_b"][0]); h["lru_rgb"] = fm(inputs["a_rg_b"][0]); h["lru_igb"] = fm(inputs["a_ig_b"][0])
    h["lru_lam"] = fm(inputs["a_lambda"][0])
    h["a_w_in"] = inputs["a_w_in"][0]
    h["a_rg_w"] = inputs["a_rg_w"][0]; h["a_ig_w"] = inputs["a_ig_w"][0]
    return h


def x_ext_for_core(x, cidx):
    xe = np.zeros((HALO + T, D), np.float32)
    xe[HALO:] = x[cidx * T:(cidx + 1) * T]
    if cidx > 0:
        xe[:HALO] = x[cidx * T - HALO:cidx * T]
    return xe


def build_A(arrays):
    nc = bass.Bass("TRN2", target_bir_lowering=False)
    dr = declare_inputs(nc, arrays)
    summ = nc.dram_tensor("summ", [128, 12, 2], F32, kind="ExternalOutput").ap()
    with ExitStack() as st:
        kb = KB(nc, st); cx = Ctx(nc, kb, st)
        c = load_consts(cx, st, dr)
        xT = cx.sb(st, "xT", [128, NCH, HALO + T], BF16); b_xT = [Buf() for _ in range(NCH)]
        with ExitStack() as s2:
            emit_load_transpose(cx, c, s2, dr["x_ext"], HALO + T, xT, b_xT, 0)
            kb.barrier()
        emit_l0_mixer(cx, c, dr, xT, b_xT, None, None, None, None, summ_dram=summ, summ_buf=Buf())
        kb.barrier()
    return nc


def build_B(arrays, upto="all", n_exp=NE):
    nc = bass.Bass("TRN2", target_bir_lowering=False)
    dr = declare_inputs(nc, arrays)
    outs = {}
    h2T = nc.dram_tensor("h2T", [D, T], F32, kind="ExternalOutput").ap()
    mix_dram = nc.dram_tensor("mix_dram", [D, T], BF16, kind="ExternalOutput" if upto == "mixer" else "Internal").ap()
    hT0 = nc.dram_tensor("hT0", [D, HALO + T], F32, kind="Internal").ap()
    h1T = nc.dram_tensor("h1T", [D, T], F32, kind="ExternalOutput" if upto == "ln1" else "Internal").ap()
    if upto == "all":
        kT_out = nc.dram_tensor("kT_out", [SB_H * 128, T], BF16, kind="ExternalOutput").ap()
        v_out = nc.dram_tensor("v_out", [T, SB_H * 128], BF16, kind="ExternalOutput").ap()
    with ExitStack() as st:
        kb = KB(nc, st); cx = Ctx(nc, kb, st)
        c = load_consts(cx, st, dr)
        b_hT0 = Buf(); b_mix = Buf(); b_h1 = Buf(); b_h2 = Buf()
        with ExitStack() as sm:
            xT = cx.sb(sm, "xT", [128, NCH, HALO + T], BF16); b_xT = [Buf() for _ in range(NCH)]
            with ExitStack() as s2:
                emit_load_transpose(cx, c, s2, dr["x_ext"], HALO + T, xT, b_xT, 0, hT_dram=hT0, hT_buf=b_hT0)
                kb.barrier()
            hin, b_hin = emit_carry(cx, c, sm, dr)
            memkv = emit_mem_kv(cx, c, sm, dr, 0)
            emit_l0_mixer(cx, c, dr, xT, b_xT, mix_dram, b_mix, hin, b_hin, memkv=memkv)
            kb.barrier()
        if upto != "mixer":
            emit_wout_ln1(cx, c, dr, 0, dr["a_w_out"], mix_dram, b_mix, hT0[:, HALO:], b_hT0, h1T, b_h1)
        if upto == "all":
            emit_moe_ln2(cx, c, dr, 0, h1T, b_h1, h2T, b_h2, n_exp=n_exp)
            emit_kv_proj(cx, c, dr, h2T, b_h2, kT_out, v_out, Buf())
        kb.barrier()
    return nc


SB_H = 12
NKB = S // 128


def emit_kv_proj(cx, c, dr, h2T, b_h2, kT_out, v_out, b_out):
    kb = cx.kb
    wkv = dr["w_kv_shared"]
    with ExitStack() as st:
        inT = cx.sb(st, "kvinT", [128, NCH, T], BF16); b_inT = [Buf() for _ in range(NCH)]
        with ExitStack() as s2:
            emit_load_fm_bf16(cx, s2, h2T, b_h2, inT, b_inT)
            kb.barrier()
        wk = alloc_wk(cx, st, "kv")
        ko = [cx.sb(st, f"kvo{k}", [128, 512], BF16) for k in range(2)]; b_ko = [Buf(), Buf()]
        cnt = [0]

        def epi(i, bi, p, bp):
            k = cnt[0] % 2; cnt[0] += 1
            kb.op("act", lambda e: e.copy(ko[k][:], p[:]), reads=[bp], writes=[b_ko[k]])
            kb.dma("sp", kT_out[i * 128:(i + 1) * 128, bi * 512:(bi + 1) * 512], ko[k][:], reads=[b_ko[k]], writes=[b_out])
        emit_proj(cx, wkv, [i * 128 for i in range(SB_H)], inT, b_inT, [(i * 512, 512) for i in range(T // 512)], epi, wk)
        stg, b_stg, wbf, b_wbf = wk
        for nb in range(SB_H):
            k = nb % 2
            kb.dma("sp", stg[k][:], wkv[:, 1536 + nb * 128:1536 + (nb + 1) * 128].rearrange("(c p) n -> p c n", p=128), writes=[b_stg[k]])
            kb.op("dve", lambda e, k=k: e.tensor_copy(wbf[k][:, 0:8, :], stg[k][:, 0:8, :]), reads=[b_stg[k]], writes=[b_wbf[k]])
            kb.op("pool", lambda e, k=k: e.tensor_copy(wbf[k][:, 8:16, :], stg[k][:, 8:16, :]), reads=[b_stg[k]], writes=[b_wbf[k]])
            for tg4 in range(T // 512):
                p, bp = cx.psum[tg4 % 4]
                for tt in range(4):
                    t0 = tg4 * 512 + tt * 128
                    for ch in range(NCH):
                        kb.op("pe", lambda e, ch=ch, p=p, tt=tt, t0=t0, k=k: e.matmul(p[:, tt * 128:(tt + 1) * 128], lhsT=inT[:, ch, t0:t0 + 128], rhs=wbf[k][:, ch, :],
                                                                                      start=(ch == 0), stop=(ch == NCH - 1)),
                              reads=[b_inT[ch], b_wbf[k]], writes=[bp])
                kk = cnt[0] % 2; cnt[0] += 1
                kb.op("act", lambda e, p=p, kk=kk: e.copy(ko[kk][:], p[:]), reads=[bp], writes=[b_ko[kk]])
                kb.dma("sp", v_out[tg4 * 512:(tg4 + 1) * 512, nb * 128:(nb + 1) * 128].rearrange("(a p) n -> p a n", p=128),
                       ko[kk][:].rearrange("p (a n) -> p a n", a=4), reads=[b_ko[kk]], writes=[b_out])
        kb.barrier()


def emit_l1_mixer(cx, c, dr, inT, b_inT, mix_dram, mix_buf, memkv, n_heads=SB_H, n_kb=NKB, kv_src=None, kv_buf=None):
    kb = cx.kb
    wq = dr["b_w_q"]
    SEG = 2048
    if kv_src is None:
        kT_all, v_all = dr["kT_all"], dr["v_all"]
        kslice = lambda h, seg: kT_all[h * 128:(h + 1) * 128, seg * SEG:(seg + 1) * SEG]
        kv_reads = []
    else:
        kT_g, v_all = kv_src
        kslice = lambda h, seg: kT_g[seg * 1536 + h * 128:seg * 1536 + (h + 1) * 128, :]
        kv_reads = [kv_buf]
    with ExitStack() as st:
        wk = alloc_wk(cx, st, "l1")
        qT = [cx.sb(st, f"sbq{k}", [128, T], BF16) for k in range(2)]; b_qT = [Buf(), Buf()]
        mo = [cx.sb(st, f"sbmo{k}", [128, T], BF16) for k in range(2)]; b_mo = [Buf(), Buf()]
        kseg = [cx.sb(st, f"kseg{k}", [128, SEG], BF16) for k in range(2)]; b_kseg = [Buf(), Buf()]
        vseg = [cx.sb(st, f"vseg{k}", [128, SEG // 128, 128], BF16) for k in range(2)]; b_vseg = [Buf(), Buf()]
        qpos = cx.sb(st, "qpos", [128, 4, 512]); kpos = cx.sb(st, "kpos", [128, NKB]); b_pos = Buf()
        negU = cx.sb(st, "negU", [128, 128], BF16); negUf = cx.sb(st, "negUf", [128, 128]); b_negU = Buf()
        negones = cx.sb(st, "negones", [128, 128]); b_negones = Buf()
        kb.dma("sp", qpos[:], dr["qpos"], writes=[b_pos])
        kb.dma("sp", kpos[:], dr["kpos"], writes=[b_pos])
        kb.dma("sp", negUf[:], dr["negU"], writes=[b_negU])
        kb.op("dve", lambda e: e.tensor_copy(negU[:], negUf[:]), reads=[b_negU], writes=[b_negU])
        kb.op("pool", lambda e: e.memset(negones[:], -1.0), writes=[b_negones])
        NT = 4
        te = [cx.sb(st, f"sb_e{k}", [128, 512]) for k in range(NT)]; b_te = [Buf() for _ in range(NT)]
        tsm = [cx.sb(st, f"sb_sm{k}", [128, 512]) for k in range(NT)]; b_tsm = [Buf() for _ in range(NT)]
        tsb = [cx.sb(st, f"sb_sb{k}", [128, 512], BF16) for k in range(NT)]; b_tsb = [Buf() for _ in range(NT)]
        tw = [cx.sb(st, f"sb_w{k}", [128, 512], BF16) for k in range(NT)]; b_tw = [Buf() for _ in range(NT)]
        spacc = [cx.sb(st, f"sb_acc{k}", [128, 512]) for k in range(3)]; b_spacc = [Buf() for _ in range(3)]
        tblocks = [(i * 512, 512) for i in range(T // 512)]
        nseg = n_kb * 128 // SEG
        seg_ctr = 0
        grp_ctr = 0
        for h in range(n_heads):
            k2 = h % 2

            def epi_q(i, bi, p, bp):
                kb.op("act", lambda e: e.mul(qT[k2][:, bi * 512:(bi + 1) * 512], p[:], 128.0 ** -0.5), reads=[bp], writes=[b_qT[k2]])
            emit_proj(cx, wq, [h * 128], inT, b_inT, tblocks, epi_q, wk, ps_banks=(6, 7))
            pairs = []
            for Q in range(T // 512):
                first = True
                j = 0
                for seg in range(nseg - 1, -1, -1):
                    for bl in range(SEG // 128 - 1, -1, -1):
                        B = seg * (SEG // 128) + bl
                        pairs.append(dict(Q=Q, B=B, seg=seg, bl=bl, first=first, last=(B == 0), segfirst=(bl == SEG // 128 - 1),
                                          ks=seg_ctr % 2, i=len(pairs), j=j, po=4 + grp_ctr % 2))
                        first = False
                        j += 1
                    seg_ctr += 1
                grp_ctr += 1

            def S1(p):
                i4 = p["i"] % NT; pZ, bZ = cx.psum[p["i"] % 3]; ks = p["ks"]; Q = p["Q"]; B = p["B"]; bl = p["bl"]; j = p["i"]
                qsl = slice(Q * 512, (Q + 1) * 512)
                if p["segfirst"]:
                    kb.dma("sp", kseg[ks][:], kslice(h, p["seg"]), reads=kv_reads, writes=[b_kseg[ks]])
                    kb.dma("sp", vseg[ks][:], v_all[p["seg"] * SEG:(p["seg"] + 1) * SEG, h * 128:(h + 1) * 128].rearrange("(a p) n -> p a n", p=128),
                           reads=kv_reads, writes=[b_vseg[ks]])
                kb.op("pe", lambda e: e.matmul(pZ[:], lhsT=kseg[ks][:, bl * 128:(bl + 1) * 128], rhs=qT[k2][:, qsl], start=True, stop=False),
                      reads=[b_kseg[ks], b_qT[k2]], writes=[bZ])
                kb.op("act", lambda e: e.activation(te[i4][:], pZ[:], AF.Exp), reads=[bZ], writes=[b_te[i4]])
                kb.op("act", lambda e: e.activation(te[i4][:], te[i4][:], AF.Ln, bias=1.0, scale=1.0), reads=[b_te[i4]], writes=[b_te[i4]])
                kb.op("dve", lambda e: e.scalar_tensor_tensor(tsm[i4][:], qpos[:, Q, :], kpos[:, B:B + 1], te[i4][:], op0=ALU.is_gt, op1=ALU.mult),
                      reads=[b_pos, b_te[i4]], writes=[b_tsm[i4]])
                kb.op("pool", lambda e: e.tensor_copy(tsb[i4][:], tsm[i4][:]), reads=[b_tsm[i4]], writes=[b_tsb[i4]])
                if not p["last"]:
                    if p["first"]:
                        kb.op("pool", lambda e: e.tensor_copy(spacc[(j + 1) % 3][:], tsm[i4][:]), reads=[b_tsm[i4]], writes=[b_spacc[(j + 1) % 3]])
                    else:
                        kb.op("pool", lambda e: e.tensor_tensor(spacc[(j + 1) % 3][:], spacc[j % 3][:], tsm[i4][:], op=ALU.add),
                              reads=[b_tsm[i4], b_spacc[j % 3]], writes=[b_spacc[(j + 1) % 3]])

            def S2(p):
                i4 = p["i"] % NT; pZ, bZ = cx.psum[p["i"] % 3]; Q = p["Q"]; B = p["B"]; j = p["i"]
                kb.op("pe", lambda e: e.matmul(pZ[:], lhsT=negU[:], rhs=tsb[i4][:], start=False, stop=p["first"]),
                      reads=[b_negU, b_tsb[i4]], writes=[bZ])
                if not p["first"]:
                    kb.op("pe", lambda e: e.matmul(pZ[:], lhsT=negones[:], rhs=spacc[j % 3][:], start=False, stop=True),
                          reads=[b_negones, b_spacc[j % 3]], writes=[bZ])
                kb.op("act", lambda e: e.activation(te[i4][:], pZ[:], AF.Exp), reads=[bZ], writes=[b_te[i4]])
                kb.op("dve", lambda e: e.scalar_tensor_tensor(tw[i4][:], qpos[:, Q, :], kpos[:, B:B + 1], te[i4][:], op0=ALU.is_gt, op1=ALU.mult),
                      reads=[b_pos, b_te[i4]], writes=[b_tw[i4]])

            def S3(p):
                i4 = p["i"] % NT; ks = p["ks"]; bl = p["bl"]; Q = p["Q"]
                pO, bO = cx.psum[p["po"]]
                kb.op("pe", lambda e: e.matmul(pO[:], lhsT=vseg[ks][:, bl, :], rhs=tw[i4][:], start=p["first"], stop=p["last"]),
                      reads=[b_vseg[ks], b_tw[i4]], writes=[bO])
                if p["last"]:
                    kb.op("act", lambda e: e.copy(mo[k2][:, Q * 512:(Q + 1) * 512], pO[:]), reads=[bO], writes=[b_mo[k2]])

            n = len(pairs)
            for i in range(n + 2):
                if i < n:
                    S1(pairs[i])
                if 0 <= i - 1 < n:
                    S2(pairs[i - 1])
                if 0 <= i - 2 < n:
                    S3(pairs[i - 2])
            kb.dma("sp", mix_dram[h * 128:(h + 1) * 128, :], mo[k2][:], reads=[b_mo[k2]], writes=[mix_buf])
        kT, vm, b_kv = memkv
        tmp = {"ex": [tw[0], tw[1]], "b_ex": [b_tw[0], b_tw[1]], "rsum": te[0], "b_rsum": b_te[0]}
        for hm in range(4):
            k2 = hm % 2

            def epi_q2(i, bi, p, bp):
                kb.op("act", lambda e: e.copy(qT[k2][:, bi * 512:(bi + 1) * 512], p[:]), reads=[bp], writes=[b_qT[k2]])
            emit_proj(cx, wq, [SB_H * 128 + hm * 128], inT, b_inT, tblocks, epi_q2, wk)
            emit_mem_attn_head(cx, c, hm, qT[k2], b_qT[k2], kT, vm, b_kv, lambda tsl, k2=k2: mo[k2][:, tsl], b_mo[k2], tmp)
            kb.dma("sp", mix_dram[(SB_H + hm) * 128:(SB_H + hm + 1) * 128, :], mo[k2][:], reads=[b_mo[k2]], writes=[mix_buf])
        kb.barrier()


def emit_out_transpose(cx, c, h2T, b_h2, out_dram, b_out):
    kb = cx.kb
    with ExitStack() as st:
        src = [cx.sb(st, f"ot_s{k}", [128, 512]) for k in range(3)]; b_src = [Buf() for _ in range(3)]
        dst = [cx.sb(st, f"ot_d{k}", [128, 4, 512]) for k in range(2)]; b_dst = [Buf(), Buf()]
        cnt = 0
        for tb in range(T // 512):
            for cg in range(NCH // 4):
                kd = (tb * 4 + cg) % 2
                for cc in range(4):
                    ch = cg * 4 + cc
                    k = cnt % 3; cnt += 1
                    kb.dma("sp", src[k][:], h2T[ch * 128:(ch + 1) * 128, tb * 512:(tb + 1) * 512], reads=[b_h2], writes=[b_src[k]])
                    p, bp = cx.psum[cnt % 4]
                    for tt in range(4):
                        kb.op("pe", lambda e, tt=tt, p=p, k=k: e.transpose(p[:, tt * 128:(tt + 1) * 128], src[k][:, tt * 128:(tt + 1) * 128], c["ident"][:]),
                              reads=[b_src[k], c["b_ident"]], writes=[bp])
                    kb.op("dve" if cc % 2 == 0 else "act",
                          (lambda e, p=p, kd=kd, cc=cc: e.tensor_copy(dst[kd][:, :, cc * 128:(cc + 1) * 128], p[:].rearrange("p (a n) -> p a n", a=4))) if cc % 2 == 0 else
                          (lambda e, p=p, kd=kd, cc=cc: e.copy(dst[kd][:, :, cc * 128:(cc + 1) * 128], p[:].rearrange("p (a n) -> p a n", a=4))),
                          reads=[bp], writes=[b_dst[kd]])
                kb.dma("sp", out_dram[tb * 512:(tb + 1) * 512, cg * 512:(cg + 1) * 512].rearrange("(a p) n -> p a n", p=128), dst[kd][:],
                       reads=[b_dst[kd]], writes=[b_out])
        kb.barrier()


def build_C(arrays, upto="all", n_exp=NE, n_heads=SB_H):
    nc = bass.Bass("TRN2", target_bir_lowering=False)
    dr = declare_inputs(nc, arrays)
    out = nc.dram_tensor("out", [T, D], F32, kind="ExternalOutput").ap()
    mix_dram = nc.dram_tensor("mix_dram", [D, T], BF16, kind="ExternalOutput" if upto == "mixer" else "Internal").ap()
    h1T = nc.dram_tensor("h1T", [D, T], F32, kind="Internal").ap()
    h2T = nc.dram_tensor("h2T", [D, T], F32, kind="Internal").ap()
    with ExitStack() as st:
        kb = KB(nc, st); cx = Ctx(nc, kb, st)
        c = load_consts(cx, st, dr)
        b_hin = Buf(); b_mix = Buf(); b_h1 = Buf(); b_h2 = Buf(); b_out = Buf()
        with ExitStack() as sm:
            inT = cx.sb(sm, "l1inT", [128, NCH, T], BF16); b_inT = [Buf() for _ in range(NCH)]
            with ExitStack() as s2:
                emit_load_fm_bf16(cx, s2, dr["hT_in"], b_hin, inT, b_inT)
                kb.barrier()
            memkv = emit_mem_kv(cx, c, sm, dr, 1)
            emit_l1_mixer(cx, c, dr, inT, b_inT, mix_dram, b_mix, memkv, n_heads=n_heads)
            kb.barrier()
        if upto != "mixer":
            emit_wout_ln1(cx, c, dr, 1, dr["b_w_out"], mix_dram, b_mix, dr["hT_in"], b_hin, h1T, b_h1)
            emit_moe_ln2(cx, c, dr, 1, h1T, b_h1, h2T, b_h2, n_exp=n_exp)
            emit_out_transpose(cx, c, h2T, b_h2, out, b_out)
        kb.barrier()
    return nc


def attn_consts(cidx):
    qp = (cidx * T + np.arange(T, dtype=np.float32)).reshape(1, 4, 512)
    qpos = np.ascontiguousarray(np.broadcast_to(qp, (128, 4, 512))).astype(np.float32)
    kpos = (np.arange(NKB, dtype=np.float32)[None, :] * 128 + np.arange(128, dtype=np.float32)[:, None]).astype(np.float32)
    j = np.arange(128)
    negU = -(j[:, None] >= j[None, :]).astype(np.float32)
    return qpos, kpos, negU


def _kernel_unfused(**inputs):
    inputs = {k: np.asarray(v) for k, v in inputs.items()}
    x = inputs["x"][0]
    cores = list(range(NCORES))
    base = dict(lru_host_inputs(inputs)); base["ident"] = np.eye(128, dtype=np.float32)
    mapsA = []
    for cidx in cores:
        m = dict(base); m["x_ext"] = x_ext_for_core(x, cidx); mapsA.append(m)
    ncA = build_A(mapsA[0])
    resA = run_bass_kernel_spmd(ncA, mapsA, core_ids=cores)
    summ_all = np.stack([r["summ"] for r in resA.results])
    del ncA, resA
    host0 = common_host_inputs(inputs, wl=0)
    mapsB = []
    for cidx in cores:
        m = dict(base); m.update(host0)
        m["x_ext"] = mapsA[cidx]["x_ext"]; m["summ_all"] = summ_all
        sel = np.zeros((128, NCORES), np.float32); sel[:, cidx] = 1.0; m["core_sel"] = sel
        m["mem"] = inputs["mem"][0]; m["mem_w_kv"] = inputs["mem_w_kv"]; m["a_w_out"] = inputs["a_w_out"][0]
        m["w_kv_shared"] = inputs["w_kv_shared"]
        mapsB.append(m)
    ncB = build_B(mapsB[0], upto="all")
    resB = run_bass_kernel_spmd(ncB, mapsB, core_ids=cores)
    h2 = [r["h2T"] for r in resB.results]
    kT_all = np.concatenate([r["kT_out"] for r in resB.results], axis=1)
    v_all = np.concatenate([r["v_out"] for r in resB.results], axis=0)
    del ncB, resB, mapsB, mapsA
    host1 = common_host_inputs(inputs, wl=1)
    mapsC = []
    for cidx in cores:
        qp, kpos, negU = attn_consts(cidx)
        m = dict(host1)
        m.update({"ident": base["ident"], "hT_in": h2[cidx], "kT_all": kT_all, "v_all": v_all, "qpos": qp, "kpos": kpos, "negU": negU,
                  "b_w_q": inputs["b_w_q"][0], "b_w_out": inputs["b_w_out"][0], "mem": inputs["mem"][0], "mem_w_kv": inputs["mem_w_kv"]})
        mapsC.append(m)
    ncC = build_C(mapsC[0], upto="all")
    resC = run_bass_kernel_spmd(ncC, mapsC, core_ids=cores)
    out = np.concatenate([r["out"] for r in resC.results], axis=0)
    return out.reshape(1, S, D).astype(np.float32)


def build_fused(arrays, n_exp=NE, n_heads=SB_H):
    nc = bass.Bass("TRN2", target_bir_lowering=False)
    dr = declare_inputs(nc, arrays)
    out = nc.dram_tensor("out", [T, D], F32, kind="ExternalOutput").ap()
    mix_dram = nc.dram_tensor("mix_dram", [D, T], BF16, kind="Internal").ap()
    hT0 = nc.dram_tensor("hT0", [D, HALO + T], F32, kind="Internal").ap()
    h1T = nc.dram_tensor("h1T", [D, T], F32, kind="Internal").ap()
    h2T = nc.dram_tensor("h2T", [D, T], F32, kind="Internal").ap()
    h3T = nc.dram_tensor("h3T", [D, T], F32, kind="Internal").ap()
    h4T = nc.dram_tensor("h4T", [D, T], F32, kind="Internal").ap()
    summ = nc.dram_tensor("summ", [128, 24], F32, kind="Internal").ap()
    summ_g = nc.dram_tensor("summ_g", [NCORES * 128, 24], F32, kind="Internal").ap()
    kT_own = nc.dram_tensor("kT_own", [SB_H * 128, T], BF16, kind="Internal").ap()
    v_own = nc.dram_tensor("v_own", [T, SB_H * 128], BF16, kind="Internal").ap()
    kT_g = nc.dram_tensor("kT_g", [NCORES * SB_H * 128, T], BF16, kind="Internal").ap()
    v_g = nc.dram_tensor("v_g", [NCORES * T, SB_H * 128], BF16, kind="Internal").ap()
    with ExitStack() as st:
        kb = KB(nc, st); cx = Ctx(nc, kb, st)
        c = load_consts(cx, st, dr)
        b_hT0 = Buf(); b_mix = Buf(); b_h1 = Buf(); b_h2 = Buf(); b_h3 = Buf(); b_h4 = Buf(); b_out = Buf()
        b_summ = Buf(); b_summg = Buf(); b_kvown = Buf(); b_kvg = Buf()
        with ExitStack() as sm:
            xT = cx.sb(sm, "xT", [128, NCH, HALO + T], BF16); b_xT = [Buf() for _ in range(NCH)]
            with ExitStack() as s2:
                emit_load_transpose(cx, c, s2, dr["x_ext"], HALO + T, xT, b_xT, 0, hT_dram=hT0, hT_buf=b_hT0)
                kb.barrier()
            emit_l0_mixer(cx, c, dr, xT, b_xT, None, None, None, None, summ_dram=summ.rearrange("p (n two) -> p n two", two=2), summ_buf=b_summ)
            kb.collective("AllGather", [summ], [summ_g], reads=[b_summ], writes=[b_summg])
            hin, b_hin = emit_carry(cx, c, sm, dr, summ_src=summ_g, summ_buf=b_summg)
            memkv = emit_mem_kv(cx, c, sm, dr, 0)
            emit_l0_mixer(cx, c, dr, xT, b_xT, mix_dram, b_mix, hin, b_hin, memkv=memkv)
            kb.barrier()
        emit_wout_ln1(cx, c, dr, 0, dr["a_w_out"], mix_dram, b_mix, hT0[:, HALO:], b_hT0, h1T, b_h1)
        emit_moe_ln2(cx, c, dr, 0, h1T, b_h1, h2T, b_h2, n_exp=n_exp, wl=0)
        emit_kv_proj(cx, c, dr, h2T, b_h2, kT_own, v_own, b_kvown)
        kb.collective("AllGather", [kT_own], [kT_g], reads=[b_kvown], writes=[b_kvg])
        kb.collective("AllGather", [v_own], [v_g], reads=[b_kvown], writes=[b_kvg])
        with ExitStack() as sm:
            inT = cx.sb(sm, "l1inT", [128, NCH, T], BF16); b_inT = [Buf() for _ in range(NCH)]
            with ExitStack() as s2:
                emit_load_fm_bf16(cx, s2, h2T, b_h2, inT, b_inT)
                kb.barrier()
            memkv = emit_mem_kv(cx, c, sm, dr, 1)
            emit_l1_mixer(cx, c, dr, inT, b_inT, mix_dram, b_mix, memkv, n_heads=n_heads, kv_src=(kT_g, v_g), kv_buf=b_kvg)
            kb.barrier()
        emit_wout_ln1(cx, c, dr, 1, dr["b_w_out"], mix_dram, b_mix, h2T, b_h2, h3T, b_h3)
        emit_moe_ln2(cx, c, dr, 1, h3T, b_h3, h4T, b_h4, n_exp=n_exp, wl=1)
        emit_out_transpose(cx, c, h4T, b_h4, out, b_out)
        kb.barrier()
    return nc


def kernel_unfused(**inputs):
    return _kernel_unfused(**inputs)


def fused_maps(inputs):
    x = inputs["x"][0]
    base = dict(lru_host_inputs(inputs)); base["ident"] = np.eye(128, dtype=np.float32)
    host = common_host_inputs(inputs)
    maps = []
    for cidx in range(NCORES):
        qp, kpos, negU = attn_consts(cidx)
        m = dict(base); m.update(host)
        sel = np.zeros((128, NCORES), np.float32); sel[:, cidx] = 1.0
        m.update({"x_ext": x_ext_for_core(x, cidx), "core_sel": sel, "mem": inputs["mem"][0], "mem_w_kv": inputs["mem_w_kv"],
                  "a_w_out": inputs["a_w_out"][0], "w_kv_shared": inputs["w_kv_shared"], "qpos": qp, "kpos": kpos, "negU": negU,
                  "b_w_q": inputs["b_w_q"][0], "b_w_out": inputs["b_w_out"][0]})
        maps.append(m)
    return maps


FUSED = False


def kernel(**inputs):
    if not FUSED:
        return _kernel_unfused(**inputs)
    inputs = {k: np.asarray(v) for k, v in inputs.items()}
    maps = fused_maps(inputs)
    nc = build_fused(maps[0])
    res = run_bass_kernel_spmd(nc, maps, core_ids=list(range(NCORES)))
    out = np.concatenate([r["out"] for r in res.results], axis=0)
    return out.reshape(1, S, D).astype(np.float32)
```
